# Optimizing a Trainium2 kernel written in Bass

```python
import math
import jax, jax.numpy as jnp
from jax import lax
import numpy as np

D_MODEL = 2048
BATCH = 4
SEQ = 4096
DEPTH = 4

A_HEAD_DIM = 128
A_HEADS = D_MODEL // (2 * A_HEAD_DIM)
A_WIDTH = A_HEADS * A_HEAD_DIM
MOBA_BLOCK = 256
MOBA_TOPK = 3
MOBA_Q_CHUNK = 32

B_HEAD_DIM = 64
B_WIDTH = D_MODEL - A_WIDTH
B_HEADS = B_WIDTH // B_HEAD_DIM
LORA_W = max(32, int(round(math.sqrt(B_WIDTH) * 1.8 / 32)) * 32)
LORA_A = max(32, int(round(math.sqrt(B_WIDTH) * 1.8 / 32)) * 32)
LORA_G = max(32, int(round(B_WIDTH ** 0.6 * 0.8 / 32)) * 32)
B_PROJ = 3 * B_WIDTH + LORA_W + LORA_A + LORA_G
AB_IN = 3 * A_WIDTH + B_PROJ
MIX_WIDTH = A_WIDTH + B_WIDTH
GN_EPS = 64e-5

POOL_WINDOWS = (2, 4, 8, 16)
POOL_GROUP = D_MODEL // len(POOL_WINDOWS)

D_FF = ((8 * D_MODEL // 3 + 255) // 256) * 256
RMS_EPS = 1e-6
N_EVEN = (DEPTH + 1) // 2
N_ODD = DEPTH // 2

kernel_name = "hybrid_moba_rwkv7_pool_macaron"


def rms_norm(x, g):
    xf = x.astype(jnp.float32)
    y = xf * lax.rsqrt(jnp.mean(xf * xf, axis=-1, keepdims=True) + RMS_EPS)
    return (y * g.astype(jnp.float32)).astype(x.dtype)


def swiglu(x, wg, wu, wd):
    return (jax.nn.silu(x @ wg) * (x @ wu)) @ wd


def token_shift(z):
    return jnp.pad(z, ((0, 0), (1, 0), (0, 0)))[:, :-1]


def moba_attention(q, k, v):
    b, s, h, dh = q.shape
    nb = -(-s // MOBA_BLOCK)
    pad = nb * MOBA_BLOCK - s
    qh = q.transpose(0, 2, 1, 3)
    kb = jnp.pad(k.transpose(0, 2, 1, 3), ((0, 0), (0, 0), (0, pad), (0, 0))).reshape(b, h, nb, MOBA_BLOCK, dh)
    vb = jnp.pad(v.transpose(0, 2, 1, 3), ((0, 0), (0, 0), (0, pad), (0, 0))).reshape(b, h, nb, MOBA_BLOCK, dh)
    k_mean = jnp.mean(kb.astype(jnp.float32), axis=3)
    topk = min(MOBA_TOPK, nb)
    scale = dh ** -0.5
    n_chunks = s // MOBA_Q_CHUNK
    bi = jnp.arange(b)[:, None, None, None]
    hi = jnp.arange(h)[None, :, None, None]

    def chunk(c):
        start = c * MOBA_Q_CHUNK
        blk = start // MOBA_BLOCK
        qc = lax.dynamic_slice_in_dim(qh, start, MOBA_Q_CHUNK, axis=2).astype(jnp.float32)
        pos = start + jnp.arange(MOBA_Q_CHUNK)
        gate = jnp.einsum('bhqd,bhnd->bhqn', qc, k_mean)
        gate = jnp.where(jnp.arange(nb) < blk, gate, -jnp.inf)
        _, sel = lax.top_k(gate, topk)
        sel_valid = sel < blk
        k_sel = kb[bi, hi, sel].astype(jnp.float32)
        v_sel = vb[bi, hi, sel].astype(jnp.float32)
        s_sel = jnp.einsum('bhqd,bhqnkd->bhqnk', qc, k_sel) * scale
        s_sel = jnp.where(sel_valid[..., None], s_sel, -jnp.inf).reshape(b, h, MOBA_Q_CHUNK, topk * MOBA_BLOCK)
        k_own = lax.dynamic_index_in_dim(kb, blk, axis=2, keepdims=False).astype(jnp.float32)
        v_own = lax.dynamic_index_in_dim(vb, blk, axis=2, keepdims=False).astype(jnp.float32)
        s_own = jnp.einsum('bhqd,bhkd->bhqk', qc, k_own) * scale
        key_pos = blk * MOBA_BLOCK + jnp.arange(MOBA_BLOCK)
        s_own = jnp.where(key_pos[None, :] <= pos[:, None], s_own, -jnp.inf)
        p = jax.nn.softmax(jnp.concatenate([s_own, s_sel], axis=-1), axis=-1)
        p_own = p[..., :MOBA_BLOCK]
        p_sel = p[..., MOBA_BLOCK:].reshape(b, h, MOBA_Q_CHUNK, topk, MOBA_BLOCK)
        out = jnp.einsum('bhqk,bhkd->bhqd', p_own, v_own) + jnp.einsum('bhqnk,bhqnkd->bhqd', p_sel, v_sel)
        return out.astype(q.dtype)

    out = lax.map(chunk, jnp.arange(n_chunks))
    return out.transpose(1, 0, 3, 2, 4).reshape(b, s, h * dh)


def rwkv7_time_mix(zb, mu, w0, w2, a0, a2, g2, k_k, k_a, r_k, gn_g, gn_b):
    b, s, _ = zb.shape
    zf = zb.astype(jnp.float32)
    zf = zf + (token_shift(zf) - zf) * mu
    r, k, v, w_lo, a_lo, g_lo = jnp.split(
        zf, [B_WIDTH, 2 * B_WIDTH, 3 * B_WIDTH, 3 * B_WIDTH + LORA_W, 3 * B_WIDTH + LORA_W + LORA_A], axis=-1)
    w = -jax.nn.softplus(-(w0 + jnp.tanh(w_lo) @ w2)) - 0.5
    decay = jnp.exp(-jnp.exp(w))
    a = jax.nn.sigmoid(a0 + a_lo @ a2)
    g = jax.nn.sigmoid(g_lo) @ g2

    def heads(t):
        return t.reshape(b, s, B_HEADS, B_HEAD_DIM)

    kk = heads(k * k_k)
    kk = kk * lax.rsqrt(jnp.maximum(jnp.sum(kk * kk, axis=-1, keepdims=True), 1e-24))
    k = k * (1.0 + (a - 1.0) * k_a)
    r_h, k_h, v_h, w_h, a_h = heads(r), heads(k), heads(v), heads(decay), heads(a)

    def step(state, inp):
        r_t, w_t, k_t, v_t, kk_t, a_t = inp
        sa = jnp.einsum('bhvk,bhk->bhv', state, -kk_t)
        state = (state * w_t[:, :, None, :] + sa[..., None] * (kk_t * a_t)[:, :, None, :]
                 + v_t[..., None] * k_t[:, :, None, :])
        return state, jnp.einsum('bhvk,bhk->bhv', state, r_t)

    xs = (jnp.moveaxis(r_h, 1, 0), jnp.moveaxis(w_h, 1, 0), jnp.moveaxis(k_h, 1, 0),
          jnp.moveaxis(v_h, 1, 0), jnp.moveaxis(kk, 1, 0), jnp.moveaxis(a_h, 1, 0))
    state0 = jnp.zeros((b, B_HEADS, B_HEAD_DIM, B_HEAD_DIM), jnp.float32)
    _, y = lax.scan(step, state0, xs)
    y = jnp.moveaxis(y, 0, 1)
    mean = jnp.mean(y, axis=-1, keepdims=True)
    var = jnp.mean(jnp.square(y - mean), axis=-1, keepdims=True)
    y = (y - mean) * lax.rsqrt(var + GN_EPS) * gn_g.reshape(B_HEADS, B_HEAD_DIM) + gn_b.reshape(B_HEADS, B_HEAD_DIM)
    y = y + jnp.sum(r_h * k_h * r_k, axis=-1, keepdims=True) * v_h
    return (y.reshape(b, s, B_WIDTH) * g).astype(zb.dtype)


def moba_rwkv_mixer(u, w_in, w_out, mu, w0, w2, a0, a2, g2, k_k, k_a, r_k, gn_g, gn_b):
    b, s, _ = u.shape
    z = u @ w_in
    qa, ka, va, zb = jnp.split(z, [A_WIDTH, 2 * A_WIDTH, 3 * A_WIDTH], axis=-1)
    hd = (b, s, A_HEADS, A_HEAD_DIM)
    y_a = moba_attention(qa.reshape(hd), ka.reshape(hd), va.reshape(hd))
    y_b = rwkv7_time_mix(zb, mu, w0, w2, a0, a2, g2, k_k, k_a, r_k, gn_g, gn_b)
    return jnp.concatenate([y_a, y_b], axis=-1) @ w_out


def pool_mixer(u, w_grp, scale):
    b, s, d = u.shape
    uf = u.astype(jnp.float32)
    cs = jnp.concatenate([jnp.zeros((b, 1, d), jnp.float32), lax.cumsum(uf, axis=1)], axis=1)
    t = jnp.arange(s)
    outs = []
    for gi, win in enumerate(POOL_WINDOWS):
        lo_c, hi_c = gi * POOL_GROUP, (gi + 1) * POOL_GROUP
        csg = cs[:, :, lo_c:hi_c]
        lo = jnp.maximum(t + 1 - win, 0)
        cnt = jnp.minimum(t + 1, win).astype(jnp.float32)[:, None]
        mean = (csg[:, 1:] - csg[:, lo]) / cnt
        outs.append((mean - uf[:, :, lo_c:hi_c]).astype(u.dtype) @ w_grp[gi])
    return jnp.concatenate(outs, axis=-1) * scale


def setup_inputs(seed: int = 0) -> dict:
    key = jax.random.key(seed)
    ks = iter(jax.random.split(key, 32))

    def nrm(shape, scale):
        return jax.random.normal(next(ks), shape, jnp.float32) * scale

    d, f = D_MODEL, D_FF
    return {
        'x': nrm((BATCH, SEQ, d), 1.0),
        'ffn1_norm': 1.0 + nrm((DEPTH, d), 0.05),
        'ffn1_wg': nrm((DEPTH, d, f), d ** -0.5),
        'ffn1_wu': nrm((DEPTH, d, f), d ** -0.5),
        'ffn1_wd': nrm((DEPTH, f, d), f ** -0.5),
        'mix_norm': 1.0 + nrm((DEPTH, d), 0.05),
        'ffn2_norm': 1.0 + nrm((DEPTH, d), 0.05),
        'ffn2_wg': nrm((DEPTH, d, f), d ** -0.5),
        'ffn2_wu': nrm((DEPTH, d, f), d ** -0.5),
        'ffn2_wd': nrm((DEPTH, f, d), f ** -0.5),
        'ab_w_in': nrm((N_EVEN, d, AB_IN), d ** -0.5),
        'ab_w_out': nrm((N_EVEN, MIX_WIDTH, d), MIX_WIDTH ** -0.5),
        'rwkv_mu': jax.random.uniform(next(ks), (N_EVEN, B_PROJ), jnp.float32),
        'rwkv_w0': jax.random.uniform(next(ks), (N_EVEN, B_WIDTH), jnp.float32, -6.5, -1.5),
        'rwkv_w2': nrm((N_EVEN, LORA_W, B_WIDTH), 0.1 * LORA_W ** -0.5),
        'rwkv_a0': nrm((N_EVEN, B_WIDTH), 0.1),
        'rwkv_a2': nrm((N_EVEN, LORA_A, B_WIDTH), LORA_A ** -0.5),
        'rwkv_g2': nrm((N_EVEN, LORA_G, B_WIDTH), LORA_G ** -0.5),
        'rwkv_k_k': 0.85 + nrm((N_EVEN, B_WIDTH), 0.05),
        'rwkv_k_a': 1.0 + nrm((N_EVEN, B_WIDTH), 0.05),
        'rwkv_r_k': nrm((N_EVEN, B_HEADS, B_HEAD_DIM), 0.1),
        'rwkv_gn_g': 1.0 + nrm((N_EVEN, B_WIDTH), 0.05),
        'rwkv_gn_b': nrm((N_EVEN, B_WIDTH), 0.01),
        'pool_w': nrm((N_ODD, len(POOL_WINDOWS), POOL_GROUP, POOL_GROUP), POOL_GROUP ** -0.5),
        'pool_scale': 1.0 + nrm((N_ODD, d), 0.1),
        'final_norm': 1.0 + nrm((d,), 0.05),
    }


def reference(x, ffn1_norm, ffn1_wg, ffn1_wu, ffn1_wd, mix_norm, ffn2_norm, ffn2_wg, ffn2_wu, ffn2_wd,
              ab_w_in, ab_w_out, rwkv_mu, rwkv_w0, rwkv_w2, rwkv_a0, rwkv_a2, rwkv_g2, rwkv_k_k, rwkv_k_a,
              rwkv_r_k, rwkv_gn_g, rwkv_gn_b, pool_w, pool_scale, final_norm):
    h = x
    for layer in range(DEPTH):
        h = h + 0.5 * swiglu(rms_norm(h, ffn1_norm[layer]), ffn1_wg[layer], ffn1_wu[layer], ffn1_wd[layer])
        u = rms_norm(h, mix_norm[layer])
        if layer % 2 == 0:
            e = layer // 2
            h = h + moba_rwkv_mixer(u, ab_w_in[e], ab_w_out[e], rwkv_mu[e], rwkv_w0[e], rwkv_w2[e],
                                    rwkv_a0[e], rwkv_a2[e], rwkv_g2[e], rwkv_k_k[e], rwkv_k_a[e],
                                    rwkv_r_k[e], rwkv_gn_g[e], rwkv_gn_b[e]).astype(h.dtype)
        else:
            o = layer // 2
            h = h + pool_mixer(u, pool_w[o], pool_scale[o]).astype(h.dtype)
        h = h + 0.5 * swiglu(rms_norm(h, ffn2_norm[layer]), ffn2_wg[layer], ffn2_wu[layer], ffn2_wd[layer])
    return rms_norm(h, final_norm)
```

```python
import contextlib
import math
import os
import numpy as np
import concourse.bass as bass
import concourse.mybir as mybir
from concourse.bass_utils import run_bass_kernel_spmd

F32 = mybir.dt.float32
BF16 = mybir.dt.bfloat16
ALU = mybir.AluOpType
AF = mybir.ActivationFunctionType
AX = mybir.AxisListType

EPOCH = 30000
D = 2048
NCH = 16
NEG = -30000.0
import os
DBG = {k: 1 for k in os.environ.get('KDBG', '').split(',') if k}


class Tag:
    __slots__ = ("name", "last_w", "readers")

    def __init__(self, name):
        self.name = name
        self.last_w = None
        self.readers = []


class Ctx:
    def __init__(self, nc, n_dma_sems=32):
        self.nc = nc
        self.stack = contextlib.ExitStack()
        self.eng_names = ["pe", "dve", "act", "pool", "sp"]
        self.prog = {e: [] for e in self.eng_names}
        self.cnt = {e: 0 for e in self.eng_names}
        self.sem = {}
        self.semid = 0
        for e in self.eng_names:
            self.sem[e] = self._new_sem("e_" + e)
        self.waited = {e: {} for e in self.eng_names}
        self.n_main_sems = n_dma_sems
        self.n_pool_sems = int(os.environ.get("KPOOLSEMS", "8"))
        self.dma_sems = [self._new_sem("d%d" % i) for i in range(n_dma_sems + self.n_pool_sems)]
        self.dma_val = [0] * (n_dma_sems + self.n_pool_sems)
        self.dma_rr = 0
        self.pool_rr = 0
        self.tags = {}
        self.same_engine_sync = {"pe": False, "dve": True, "act": True, "pool": True, "sp": False}
        self.n_instr = 0
        self.last_ev = {}

    def _new_sem(self, name):
        self.semid += 1
        return self.stack.enter_context(self.nc.semaphore("%s_%d" % (name, self.semid)))

    def tag(self, name):
        t = self.tags.get(name)
        if t is None:
            t = Tag(name)
            self.tags[name] = t
        return t

    def _tags(self, lst):
        return [x if isinstance(x, Tag) else self.tag(x) for x in lst]

    def _deps(self, e, R, W, ses=False):
        deps = []
        for t in R:
            if t.last_w is not None:
                deps.append(t.last_w)
        for t in W:
            if t.last_w is not None:
                deps.append(t.last_w)
            deps.extend(t.readers)
        best = {}
        for (s, v, src) in deps:
            if src == e and not (self.same_engine_sync[e] or ses):
                continue
            k = id(s)
            if k not in best or best[k][1] < v:
                best[k] = (s, v)
        waits = []
        for k, (s, v) in best.items():
            if self.waited[e].get(k, 0) >= v:
                continue
            self.waited[e][k] = v
            waits.append((s, v))
        return waits

    def _commit(self, ev, R, W):
        for t in W:
            t.last_w = ev
            t.readers = []
        for t in R:
            if t not in W:
                t.readers.append(ev)
                if len(t.readers) > 64:
                    best = {}
                    for (s, v, src) in t.readers:
                        k = id(s)
                        if k not in best or best[k][1] < v:
                            best[k] = (s, v, src)
                    t.readers = list(best.values())
        self.n_instr += 1

    def op(self, e, fn, reads=(), writes=(), ses=False):
        pr = [x for x in reads if isinstance(x, str) and x.startswith("ps") and x[2:].isdigit()]
        if pr:
            reads = [x for x in reads if x not in pr]
            writes = list(writes) + [x for x in pr if x not in writes]
        R = self._tags(reads)
        W = self._tags(writes)
        waits = self._deps(e, R, W, ses)
        if self.cnt[e] >= EPOCH:
            self.sem[e] = self._new_sem("e_" + e)
            self.cnt[e] = 0
        self.cnt[e] += 1
        s = self.sem[e]
        ev = (s, self.cnt[e], e)
        self.prog[e].append((waits, fn, (s, 1)))
        self.last_ev[e] = ev
        self._commit(ev, R, W)
        return ev

    def I(self, e, method, *args, reads=(), writes=(), ses=False, **kw):
        return self.op(e, lambda eng: getattr(eng, method)(*args, **kw), reads=reads, writes=writes, ses=ses)

    def dma(self, q, out, in_, reads=(), writes=()):
        R = self._tags(reads)
        W = self._tags(writes)
        waits = self._deps(q, R, W)
        if q == "pool":
            i = self.n_main_sems + self.pool_rr
            self.pool_rr = (self.pool_rr + 1) % self.n_pool_sems
        else:
            i = self.dma_rr
            self.dma_rr = (self.dma_rr + 1) % self.n_main_sems
        s = self.dma_sems[i]
        if self.dma_val[i] > 0 and self.waited[q].get(id(s), 0) < self.dma_val[i]:
            waits.append((s, self.dma_val[i]))
            self.waited[q][id(s)] = self.dma_val[i]
        self.dma_val[i] += 16
        ev = (s, self.dma_val[i], "dma")

        def fn(eng, out=out, in_=in_):
            return eng.dma_start(out=out, in_=in_)

        self.prog[q].append((waits, fn, (s, 16)))
        self._commit(ev, R, W)
        return ev

    def barrier(self):
        evs = [(s, v) for (s, v, _e) in self.last_ev.values()]
        for i, s in enumerate(self.dma_sems[:self.n_main_sems]):
            if self.dma_val[i] > 0:
                evs.append((s, self.dma_val[i]))
        for e in self.eng_names:
            waits = []
            for (s, v) in evs:
                if s is self.sem[e] and not self.same_engine_sync[e]:
                    continue
                if self.waited[e].get(id(s), 0) >= v:
                    continue
                self.waited[e][id(s)] = v
                waits.append((s, v))
            if waits:
                self.prog[e].append((waits, None, None))

    def finish(self, final_events):
        nc = self.nc
        for (s, v, _src) in final_events:
            self.prog["sp"].append(([(s, v)], None, None))
        engmap = {"pe": "tensor", "dve": "vector", "act": "scalar", "pool": "gpsimd", "sp": "sync"}
        with nc.Block() as block:
            for e in self.eng_names:
                plist = self.prog[e]

                def body(eng, plist=plist):
                    for (waits, fn, inc) in plist:
                        for (s, v) in waits:
                            eng.wait_ge(s, v)
                        if fn is not None:
                            fn(eng).then_inc(inc[0], inc[1])

                getattr(block, engmap[e])(body)
        self.stack.close()


class Arena:
    def __init__(self, t, nwords):
        self.t = t
        self.nwords = nwords
        self.off = 0

    def reset(self, off=0):
        self.off = off

    def f32(self, *free):
        n = int(np.prod(free))
        assert self.off + n <= self.nwords, ("arena overflow", self.off, n, self.nwords)
        ap = self.t[:, self.off:self.off + n]
        self.off += n
        return _shape(ap, free)

    def bf16(self, *free):
        n = int(np.prod(free))
        n4 = (n + 1) // 2
        assert self.off + n4 <= self.nwords, ("arena overflow", self.off, n4, self.nwords)
        ap = self.t[:, self.off:self.off + n4].bitcast(BF16)
        self.off += n4
        if n4 * 2 != n:
            ap = ap[:, 0:n]
        return _shape(ap, free)


def _shape(ap, free):
    if len(free) == 1:
        return ap
    if len(free) == 2:
        return ap.rearrange("p (a b) -> p a b", b=free[1])
    if len(free) == 3:
        return ap.rearrange("p (a b c) -> p a b c", b=free[1], c=free[2])
    raise ValueError(free)


def make_consts():
    c = {}
    c["ident"] = np.eye(128, dtype=np.float32)
    k = np.arange(128)[:, None]
    q = np.arange(128)[None, :]
    c["tri"] = np.where(k <= q, 0.0, NEG).astype(np.float32)
    same = (k // 64) == (q // 64)
    c["bones"] = same.astype(np.float32)
    c["maskU"] = (same & (k < q)).astype(np.float32)
    c["maskUe"] = (same & (k <= q)).astype(np.float32)
    c["maskL"] = (same & (k > q)).astype(np.float32)
    reset = np.ones((128, 512), np.float32)
    reset[:, ::64] = 0.0
    c["reset"] = reset
    inv = np.zeros((128, 4, 16), np.float32)
    for gi, w in enumerate((2, 4, 8, 16)):
        inv[:, gi, :] = 1.0 / np.minimum(np.arange(16) + 1, w)
    c["invcnt"] = inv.reshape(128, 64)
    order = ["ident", "tri", "bones", "maskU", "maskUe", "maskL", "reset", "invcnt"]
    offs = {}
    o = 0
    for n in order:
        offs[n] = (o, c[n].shape[1])
        o += c[n].shape[1]
    return np.concatenate([c[n] for n in order], axis=1), offs


CONSTS, COFF = make_consts()


class Cfg:
    def __init__(self, S=4096, depth=4, dff=5632, TT=512, stages=None):
        self.S = S
        self.depth = depth
        self.dff = dff
        self.TT = TT
        self.NT = S // TT
        self.NJ = dff // 128
        self.JB = dff // 512
        self.JG = 4
        self.JQ = self.NJ // self.JG
        self.n_even = (depth + 1) // 2
        self.n_odd = depth // 2
        self.stages = stages


class Builder:
    def __init__(self, cfg):
        self.cfg = cfg
        self.nc = bass.Bass("TRN2", target_bir_lowering=False)
        self.es = contextlib.ExitStack()

    def declare(self):
        nc, cfg = self.nc, self.cfg
        S, dp, dff = cfg.S, cfg.depth, cfg.dff
        ne, no = cfg.n_even, max(cfg.n_odd, 1)

        def din(name, shape):
            return nc.dram_tensor(name, list(shape), F32, kind="ExternalInput").ap()

        self.xT = din("xT", [D, S])
        self.inp = {}
        for n, shp in (("ffn1_norm", [dp, D]), ("ffn1_wg", [dp, D, dff]), ("ffn1_wu", [dp, D, dff]),
                       ("ffn1_wd", [dp, dff, D]), ("mix_norm", [dp, D]), ("ffn2_norm", [dp, D]),
                       ("ffn2_wg", [dp, D, dff]), ("ffn2_wu", [dp, D, dff]), ("ffn2_wd", [dp, dff, D]),
                       ("ab_w_in", [ne, D, 6336]), ("ab_w_out", [ne, D, D]), ("rwkv_mu", [ne, 3264]),
                       ("rwkv_w0", [ne, 1024]), ("rwkv_w2", [ne, 64, 1024]), ("rwkv_a0", [ne, 1024]),
                       ("rwkv_a2", [ne, 64, 1024]), ("rwkv_g2", [ne, 64, 1024]), ("rwkv_k_k", [ne, 1024]),
                       ("rwkv_k_a", [ne, 1024]), ("rwkv_r_k", [ne, 16, 64]), ("rwkv_gn_g", [ne, 1024]),
                       ("rwkv_gn_b", [ne, 1024]), ("pool_w", [no, 4, 512, 512]), ("pool_scale", [no, D]),
                       ("final_norm", [D])):
            self.inp[n] = din(n, shp)
        self.consts = din("consts", list(CONSTS.shape))
        self.outT = nc.dram_tensor("outT", [D, S], F32, kind="ExternalOutput").ap()

        def dint(name, shape, dt):
            return nc.dram_tensor(name, list(shape), dt, kind="Internal").ap()

        self.hT = dint("hT", [D, S], F32)
        self.uT = dint("uT", [D, S], BF16)
        self.yT = dint("yT", [D, S], BF16)
        self.WG = {}
        self.WU = {}
        self.WD = {}
        for l in range(dp):
            for s in (1, 2):
                self.WG[(l, s)] = dint("WG_%d_%d" % (l, s), [cfg.JB, 128, 16 * 512], BF16)
                self.WU[(l, s)] = dint("WU_%d_%d" % (l, s), [cfg.JB, 128, 16 * 512], BF16)
                self.WD[(l, s)] = dint("WD_%d_%d" % (l, s), [4, cfg.JG, 128, cfg.JQ * 512], BF16)
        self.WIN = [dint("WIN_%d" % e, [13, 128, 16 * 512], BF16) for e in range(ne)]
        self.WOUT = [dint("WOUT_%d" % e, [4, 128, 16 * 512], BF16) for e in range(ne)]
        self.PW = [dint("PW_%d" % o, [128, 16 * 512], BF16) for o in range(cfg.n_odd)]

    def ew(self):
        self._ew = getattr(self, "_ew", 0) + 1
        return "dve" if self._ew % 2 else "pool"

    def dq(self):
        return "sp"

    def prep(self):
        c, cfg = self.c, self.cfg

        def conv(src, dst, tag):
            if len(src.shape) == 3:
                dst = dst.rearrange("p (a b) -> p a b", b=src.shape[2])
            c.dma("pool", dst, src, writes=[tag])

        def ffn(l, s_):
            wg = self.inp["ffn%d_wg" % s_][l].rearrange("(c p) f -> p c f", p=128)
            wu = self.inp["ffn%d_wu" % s_][l].rearrange("(c p) f -> p c f", p=128)
            for jb in range(cfg.JB):
                conv(wg[:, :, jb * 512:(jb + 1) * 512], self.WG[(l, s_)][jb], "WG%d%d_%d" % (l, s_, jb))
                conv(wu[:, :, jb * 512:(jb + 1) * 512], self.WU[(l, s_)][jb], "WU%d%d_%d" % (l, s_, jb))
            wd = self.inp["ffn%d_wd" % s_][l].rearrange("(jg jq p) (mb d) -> mb jg p jq d", jq=cfg.JQ, p=128, d=512)
            for mb in range(4):
                for jg in range(cfg.JG):
                    conv(wd[mb, jg], self.WD[(l, s_)][mb, jg], "WD%d%d_%d_%d" % (l, s_, mb, jg))

        def mix(e):
            win = self.inp["ab_w_in"][e].rearrange("(c p) n -> p c n", p=128)
            for cb in range(12):
                conv(win[:, :, cb * 512:(cb + 1) * 512], self.WIN[e][cb], "WIN%d_%d" % (e, cb))
            conv(win[:, :, 6144:6336], self.WIN[e][12][:, 0:16 * 192], "WIN%d_12" % e)
            wo = self.inp["ab_w_out"][e].rearrange("(c p) n -> p c n", p=128)
            for mb in range(4):
                conv(wo[:, :, mb * 512:(mb + 1) * 512], self.WOUT[e][mb], "WOUT%d_%d" % (e, mb))

        def pool(o):
            pw = self.inp["pool_w"][o].rearrange("g (ci p) co -> p g ci co", p=128)
            for g in range(4):
                conv(pw[:, g], self.PW[o][:, g * 2048:(g + 1) * 2048], "PW%d" % o)

        if cfg.depth >= 1:
            ffn(0, 1)
        if cfg.n_even >= 1:
            mix(0)
        for l in range(cfg.depth):
            if l > 0:
                ffn(l, 1)
                if l % 2 == 0 and l // 2 < cfg.n_even:
                    mix(l // 2)
            if l % 2 == 1 and l // 2 < cfg.n_odd:
                pool(l // 2)
            ffn(l, 2)

    def t_alloc(self):
        A, cfg = self.arena, self.cfg
        TT = cfg.TT
        A.reset(self.const_words)
        self.hs = A.f32(NCH, TT)
        self.ub = A.bf16(NCH, TT)
        self.xb = A.bf16(NCH, TT)
        self.aT = A.bf16(cfg.NJ, TT)
        self.wb = [A.bf16(8192) for _ in range(4)]
        self.wrr = 0
        self.sq = [A.f32(TT) for _ in range(2)]
        self.rstd = A.f32(TT)
        self.sg = [A.f32(TT) for _ in range(2)]
        self.uext = [A.f32(TT + 15) for _ in range(2)]
        self.ptmp = [A.f32(TT + 15) for _ in range(2)]
        self.halo = A.f32(NCH, 15)
        self.ost = A.f32(TT)

    def next_wb(self):
        i = self.wrr % 4
        self.wrr += 1
        return i

    def t_norm_stats(self, n):
        c = self.c
        for ch in range(NCH):
            sq = self.sq[ch % 2]
            c.op("act", lambda e, sq=sq, ch=ch: e.activation(out=sq[:, 0:n], in_=self.hs[:, ch, 0:n], func=AF.Square),
                 reads=["hs%d" % ch], writes=["sq%d" % (ch % 2)])
            c.op("pe", lambda e, sq=sq, ch=ch: e.matmul(self.ps[0][:, 0:n], self.onesD, sq[:, 0:n], start=(ch == 0), stop=(ch == NCH - 1)),
                 reads=["sq%d" % (ch % 2)], writes=["ps0"])
        c.op("act", lambda e: e.activation(out=self.rstd[:, 0:n], in_=self.ps[0][:, 0:n], func=AF.Sqrt, bias=self.epsc[:, 0:1], scale=1.0),
             reads=["ps0"], writes=["rstd"])
        c.op("dve", lambda e: e.reciprocal(out=self.rstd[:, 0:n], in_=self.rstd[:, 0:n]), reads=["rstd"], writes=["rstd"])

    def t_norm_apply(self, n, gidx, dst, dtag):
        c = self.c
        for ch in range(NCH):
            eng = "dve"
            c.op(eng, lambda e, ch=ch: e.scalar_tensor_tensor(out=dst[:, ch, 0:n], in0=self.hs[:, ch, 0:n], scalar=self.gains[:, gidx, ch:ch + 1],
                                                            in1=self.rstd[:, 0:n], op0=ALU.mult, op1=ALU.mult),
                 reads=["hs%d" % ch, "rstd"], writes=["%s%d" % (dtag, ch)])

    def t_ffn(self, n, l, s):
        c, cfg = self.c, self.cfg
        self.t_norm_stats(n)
        self.t_norm_apply(n, self.gidx[("ffn%d" % s, l)], self.ub, "ub")
        WG, WU, WD = self.WG[(l, s)], self.WU[(l, s)], self.WD[(l, s)]
        ubtags = ["ub%d" % ch for ch in range(NCH)]
        for jb in range(cfg.JB):
            ig = self.next_wb()
            iu = self.next_wb()
            wg = self.wb[ig].rearrange("p (c f) -> p c f", f=512)
            wu = self.wb[iu].rearrange("p (c f) -> p c f", f=512)
            c.dma("sp", self.wb[ig], WG[jb], reads=["WG%d%d_%d" % (l, s, jb)], writes=["wb%d" % ig])
            c.dma("sp", self.wb[iu], WU[jb], reads=["WU%d%d_%d" % (l, s, jb)], writes=["wb%d" % iu])
            for jj in range(4):
                j = jb * 4 + jj
                pg = j % 2
                pu = 2 + j % 2
                for ch in range(NCH):
                    c.op("pe", lambda e, ch=ch, jj=jj, pg=pg, wg=wg: e.matmul(self.ps[pg][:, 0:n], wg[:, ch, jj * 128:(jj + 1) * 128], self.ub[:, ch, 0:n],
                                                                             start=(ch == 0), stop=(ch == NCH - 1)),
                         reads=["wb%d" % ig, "ub%d" % ch], writes=["ps%d" % pg])
                for ch in range(NCH):
                    c.op("pe", lambda e, ch=ch, jj=jj, pu=pu, wu=wu: e.matmul(self.ps[pu][:, 0:n], wu[:, ch, jj * 128:(jj + 1) * 128], self.ub[:, ch, 0:n],
                                                                             start=(ch == 0), stop=(ch == NCH - 1)),
                         reads=["wb%d" % iu, "ub%d" % ch], writes=["ps%d" % pu])
                sg = self.sg[j % 2]
                c.op("act", lambda e, sg=sg, pg=pg: e.activation(out=sg[:, 0:n], in_=self.ps[pg][:, 0:n], func=AF.Silu),
                     reads=["ps%d" % pg], writes=["sg%d" % (j % 2)])
                c.op("dve", lambda e, sg=sg, pu=pu, j=j: e.tensor_tensor(out=self.aT[:, j, 0:n], in0=self.ps[pu][:, 0:n], in1=sg[:, 0:n], op=ALU.mult),
                     reads=["ps%d" % pu, "sg%d" % (j % 2)], writes=["aT%d" % j])
        for mb in range(4):
            for jg in range(cfg.JG):
                iw = self.next_wb()
                wd = self.wb[iw][:, 0:cfg.JQ * 512].rearrange("p (j d) -> p j d", d=512)
                c.dma(self.dq(), self.wb[iw][:, 0:cfg.JQ * 512], WD[mb, jg], reads=["WD%d%d_%d_%d" % (l, s, mb, jg)], writes=["wb%d" % iw])
                for jq in range(cfg.JQ):
                    j = jg * cfg.JQ + jq
                    for mi in range(4):
                        c.op("pe", lambda e, jq=jq, mi=mi, j=j, wd=wd: e.matmul(self.ps[4 + mi][:, 0:n], wd[:, jq, mi * 128:(mi + 1) * 128], self.aT[:, j, 0:n],
                                                                               start=(j == 0), stop=(j == cfg.NJ - 1)),
                             reads=["wb%d" % iw, "aT%d" % j], writes=["ps%d" % (4 + mi)])
            for mi in range(4):
                m = mb * 4 + mi
                c.op("dve", lambda e, mi=mi, m=m: e.scalar_tensor_tensor(out=self.hs[:, m, 0:n], in0=self.ps[4 + mi][:, 0:n], scalar=0.5, in1=self.hs[:, m, 0:n],
                                                                        op0=ALU.mult, op1=ALU.add),
                     reads=["ps%d" % (4 + mi), "hs%d" % m], writes=["hs%d" % m])

    def t_wout(self, n, e_idx, t0):
        c = self.c
        ysrc = self.yT.rearrange("(c p) s -> p c s", p=128)[:, :, t0:t0 + n]
        c.dma("sp", self.xb[:, :, 0:n], ysrc, reads=["yT"], writes=["xb%d" % ch for ch in range(NCH)])
        for mb in range(4):
            iw = self.next_wb()
            w = self.wb[iw].rearrange("p (c f) -> p c f", f=512)
            c.dma(self.dq(), self.wb[iw], self.WOUT[e_idx][mb], reads=["WOUT%d_%d" % (e_idx, mb)], writes=["wb%d" % iw])
            for mi in range(4):
                for ch in range(NCH):
                    c.op("pe", lambda e, ch=ch, mi=mi, w=w: e.matmul(self.ps[4 + mi][:, 0:n], w[:, ch, mi * 128:(mi + 1) * 128], self.xb[:, ch, 0:n],
                                                                    start=(ch == 0), stop=(ch == NCH - 1)),
                         reads=["wb%d" % iw, "xb%d" % ch], writes=["ps%d" % (4 + mi)])
                m = mb * 4 + mi
                c.op("dve", lambda e, mi=mi, m=m: e.tensor_tensor(out=self.hs[:, m, 0:n], in0=self.ps[4 + mi][:, 0:n], in1=self.hs[:, m, 0:n], op=ALU.add),
                     reads=["ps%d" % (4 + mi), "hs%d" % m], writes=["hs%d" % m])

    def t_pool(self, n, l, first_tile):
        c = self.c
        o = l // 2
        self.t_norm_stats(n)
        gidx = self.gidx[("mix", l)]
        iw = self.next_wb()
        pw = self.wb[iw].rearrange("p (g ci co) -> p g ci co", ci=4, co=512)
        c.dma(self.dq(), self.wb[iw], self.PW[o], reads=["PW%d" % o], writes=["wb%d" % iw])
        for gi, win in enumerate((2, 4, 8, 16)):
            for ci in range(4):
                ch = gi * 4 + ci
                ue = self.uext[ch % 2]
                utag = "uext%d" % (ch % 2)
                if first_tile:
                    c.op("pool", lambda e, ue=ue: e.memset(ue[:, 0:15], 0.0), writes=[utag])
                else:
                    c.op("pool", lambda e, ue=ue, ch=ch: e.tensor_copy(out=ue[:, 0:15], in_=self.halo[:, ch, :]), reads=["halo%d" % ch], writes=[utag])
                c.op("dve", lambda e, ue=ue, ch=ch: e.scalar_tensor_tensor(out=ue[:, 15:15 + n], in0=self.hs[:, ch, 0:n], scalar=self.gains[:, gidx, ch:ch + 1],
                                                                          in1=self.rstd[:, 0:n], op0=ALU.mult, op1=ALU.mult),
                     reads=["hs%d" % ch, "rstd", utag], writes=[utag])
                c.op("pool", lambda e, ue=ue, ch=ch: e.tensor_copy(out=self.halo[:, ch, :], in_=ue[:, n:n + 15]), reads=[utag], writes=["halo%d" % ch])
                src, stag = ue, utag
                step = 1
                k = 0
                while step < win:
                    dst = self.ptmp[k % 2]
                    dtag = "ptmp%d" % (k % 2)
                    lo = 2 * step - 1
                    eng = self.ew()
                    c.op(eng, lambda e, src=src, dst=dst, lo=lo, step=step: e.tensor_tensor(out=dst[:, lo:15 + n], in0=src[:, lo:15 + n], in1=src[:, lo - step:15 + n - step], op=ALU.add),
                         reads=[stag], writes=[dtag])
                    src, stag = dst, dtag
                    step *= 2
                    k += 1
                eng = "dve"
                c.op(eng, lambda e, src=src, ue=ue, ch=ch, win=win: e.scalar_tensor_tensor(out=self.xb[:, ch, 0:n], in0=src[:, 15:15 + n], scalar=1.0 / win, in1=ue[:, 15:15 + n],
                                                                                          op0=ALU.mult, op1=ALU.subtract),
                     reads=[stag, utag], writes=["xb%d" % ch])
                if first_tile:
                    m = min(16, n)
                    fx = self.ptmp[k % 2]
                    ftag = "ptmp%d" % (k % 2)
                    c.op("dve", lambda e, src=src, fx=fx, gi=gi, m=m: e.tensor_tensor(out=fx[:, 0:m], in0=src[:, 15:15 + m], in1=self.invcnt[:, gi, 0:m], op=ALU.mult),
                         reads=[stag], writes=[ftag])
                    c.op("dve", lambda e, fx=fx, ue=ue, ch=ch, m=m: e.tensor_tensor(out=self.xb[:, ch, 0:m], in0=fx[:, 0:m], in1=ue[:, 15:15 + m], op=ALU.subtract),
                         reads=[ftag, utag, "xb%d" % ch], writes=["xb%d" % ch])
            for co in range(4):
                for ci in range(4):
                    c.op("pe", lambda e, gi=gi, ci=ci, co=co: e.matmul(self.ps[4 + co][:, 0:n], pw[:, gi, ci, co * 128:(co + 1) * 128], self.xb[:, gi * 4 + ci, 0:n],
                                                                      start=(ci == 0), stop=(ci == 3)),
                         reads=["wb%d" % iw, "xb%d" % (gi * 4 + ci)], writes=["ps%d" % (4 + co)])
            for co in range(4):
                m = gi * 4 + co
                c.op("dve", lambda e, co=co, m=m: e.scalar_tensor_tensor(out=self.hs[:, m, 0:n], in0=self.ps[4 + co][:, 0:n], scalar=self.pscale[:, o, m:m + 1],
                                                                        in1=self.hs[:, m, 0:n], op0=ALU.mult, op1=ALU.add),
                     reads=["ps%d" % (4 + co), "hs%d" % m], writes=["hs%d" % m])


    def m_alloc_moba(self):
        A, cfg = self.arena, self.cfg
        S = cfg.S
        A.reset(self.const_words)
        self.QT = A.bf16(4, S)
        self.KT = A.bf16(4, S)
        self.V = A.bf16(S // 128, 512)
        self.maskT = A.bf16(S)
        self.sel = A.bf16(64, 128)
        self.mut = A.bf16(NCH, 512)
        self.wbm = [A.bf16(8192) for _ in range(2)]
        self.qf = A.f32(4, 512)
        self.kmT = A.f32(4, 16)
        self.gs = A.f32(64)
        self.mk = A.f32(64)
        self.mx = A.f32(32)
        self.pT = [A.bf16(256) for _ in range(4)]
        self.rc = A.f32(256)
        self.yo = [A.bf16(256) for _ in range(2)]
        self.identB = A.bf16(128)
        self.triB = A.bf16(128)
        self.onesB = A.bf16(128)
        c = self.c
        c.op("dve", lambda e: e.tensor_copy(out=self.identB, in_=self.ident), reads=["cst"], writes=["mconst"])
        c.op("dve", lambda e: e.tensor_copy(out=self.triB, in_=self.tri_f), reads=["cst"], writes=["mconst"])
        c.op("pool", lambda e: e.memset(self.onesB, 1.0), writes=["mconst"])
        c.op("dve", lambda e: e.tensor_copy(out=self.sel[0:64], in_=self.ident[0:64, 0:64].unsqueeze(2).to_broadcast([64, 64, 128])),
             reads=["cst"], writes=["mconst"])
        c.op("pool", lambda e: e.memset(self.kmT, 0.0), writes=["kmT"])

    def m_moba(self, e_idx):
        c, cfg = self.c, self.cfg
        S, NT = cfg.S, cfg.S // 512
        NB = S // 256
        WIN = self.WIN[e_idx]
        uTv = self.uT.rearrange("(c p) s -> p c s", p=128)
        scale = 128.0 ** -0.5
        allut = ["ut%d" % ch for ch in range(NCH)]
        wrr = [0]

        def loadw(cb):
            i = wrr[0] % 2
            wrr[0] += 1
            c.dma("sp", self.wbm[i], WIN[cb], reads=["WIN%d_%d" % (e_idx, cb)], writes=["wbm%d" % i])
            return i, self.wbm[i].rearrange("p (c f) -> p c f", f=512)

        for hg in range(2):
            for t in range(NT):
                t0 = t * 512
                c.dma("sp", self.mut, uTv[:, :, t0:t0 + 512], reads=["uT"], writes=allut)
                iq, wq = loadw(hg)
                for hh in range(4):
                    pb = hh % 2
                    for ch in range(NCH):
                        c.op("pe", lambda e, ch=ch, hh=hh, pb=pb, wq=wq: e.matmul(self.ps[pb][:, 0:512], wq[:, ch, hh * 128:(hh + 1) * 128], self.mut[:, ch, :],
                                                                                 start=(ch == 0), stop=(ch == NCH - 1)),
                             reads=["wbm%d" % iq, "ut%d" % ch], writes=["ps%d" % pb])
                    c.op("act", lambda e, hh=hh, pb=pb, t0=t0: e.activation(out=self.QT[:, hh, t0:t0 + 512], in_=self.ps[pb][:, 0:512], func=AF.Copy, scale=scale),
                         reads=["ps%d" % pb], writes=["QT%d" % hh])
                    c.op("dve", lambda e, hh=hh, pb=pb: e.tensor_copy(out=self.qf[:, hh, :], in_=self.ps[pb][:, 0:512]),
                         reads=["ps%d" % pb], writes=["qf"])
                ik, wk = loadw(2 + hg)
                for hh in range(4):
                    pb = 2 + hh % 2
                    for ch in range(NCH):
                        c.op("pe", lambda e, ch=ch, hh=hh, pb=pb, wk=wk: e.matmul(self.ps[pb][:, 0:512], wk[:, ch, hh * 128:(hh + 1) * 128], self.mut[:, ch, :],
                                                                                 start=(ch == 0), stop=(ch == NCH - 1)),
                             reads=["wbm%d" % ik, "ut%d" % ch], writes=["ps%d" % pb])
                    c.op("act", lambda e, hh=hh, pb=pb, t0=t0: e.activation(out=self.KT[:, hh, t0:t0 + 512], in_=self.ps[pb][:, 0:512], func=AF.Copy),
                         reads=["ps%d" % pb], writes=["KT%d" % hh])
                    c.op("dve", lambda e, hh=hh, pb=pb, t=t: e.reduce_sum(out=self.kmT[:, hh, 2 * t:2 * t + 2], in_=self.ps[pb][:, 0:512].rearrange("p (b k) -> p b k", k=256), axis=AX.X),
                         reads=["ps%d" % pb], writes=["kmT"])
                iv, wv = loadw(4 + hg)
                for sub in range(4):
                    pb = 4 + sub % 2
                    for ch in range(NCH):
                        c.op("pe", lambda e, ch=ch, sub=sub, pb=pb, wv=wv: e.matmul(self.ps[pb][:, 0:512], self.mut[:, ch, sub * 128:(sub + 1) * 128], wv[:, ch, :],
                                                                                   start=(ch == 0), stop=(ch == NCH - 1)),
                             reads=["wbm%d" % iv, "ut%d" % ch], writes=["ps%d" % pb])
                    c.op("act", lambda e, sub=sub, pb=pb, t=t: e.activation(out=self.V[:, 4 * t + sub, :], in_=self.ps[pb][:, 0:512], func=AF.Copy),
                         reads=["ps%d" % pb], writes=["V"])
                for sub in range(4):
                    blk = 2 * t + sub // 2
                    if blk == 0 or DBG.get("nogate"):
                        continue
                    for hh in range(4):
                        c.op("pe", lambda e, hh=hh, sub=sub: e.matmul(self.ps[6][:, hh * 16:(hh + 1) * 16], self.qf[:, hh, sub * 128:(sub + 1) * 128], self.kmT[:, hh, :],
                                                                     start=True, stop=True),
                             reads=["qf", "kmT"], writes=["ps6"])
                    c.op("pool", lambda e: e.memset(self.gs, -1e30), writes=["gs"])
                    c.op("dve", lambda e, blk=blk: e.tensor_copy(out=self.gs.rearrange("p (h n) -> p h n", n=16)[:, :, 0:blk],
                                                                in_=self.ps[6][:, 0:64].rearrange("p (h n) -> p h n", n=16)[:, :, 0:blk]),
                         reads=["ps6", "gs"], writes=["gs"])
                    for hh in range(4):
                        c.op("dve", lambda e, hh=hh: e.max(out=self.mx[:, hh * 8:(hh + 1) * 8], in_=self.gs[:, hh * 16:(hh + 1) * 16]),
                             reads=["gs"], writes=["mx"])
                    for hh in range(4):
                        c.op("dve", lambda e, hh=hh: e.tensor_scalar(out=self.mk[:, hh * 16:(hh + 1) * 16], in0=self.gs[:, hh * 16:(hh + 1) * 16],
                                                                    scalar1=self.mx[:, hh * 8 + 2:hh * 8 + 3], scalar2=1.0, op0=ALU.is_ge, op1=ALU.subtract),
                             reads=["gs", "mx"], writes=["mk"])
                    c.op("pe", lambda e: e.matmul(self.ps[7][0:64, 0:128], self.mk, self.ident, start=True, stop=True),
                         reads=["mk", "cst"], writes=["ps7"])
                    q0 = t0 + sub * 128
                    c.op("act", lambda e, q0=q0: e.activation(out=self.maskT[0:64, q0:q0 + 128], in_=self.ps[7][0:64, 0:128], func=AF.Copy, scale=-NEG),
                         reads=["ps7"], writes=["maskT"])
            I = c.I
            PCB = [0, 1, 6, 7]
            steps = []
            for hh in range(0 if DBG.get("noattn") else 4):
                for b in range(NB):
                    for kt in range(2 * b + 2):
                        steps.append((hh, b, kt))

            def emit_qk(i):
                hh, b, kt = steps[i]
                pc = PCB[i % 4]
                q0 = b * 256
                k0 = kt * 128
                nb = kt // 2
                if nb < b:
                    I("pe", "matmul", self.ps[pc][:, 0:256], self.KT[:, hh, k0:k0 + 128], self.QT[:, hh, q0:q0 + 256], start=True, stop=False,
                      reads=["KT%d" % hh, "QT%d" % hh], writes=["ps%d" % pc])
                    r = hh * 16 + nb
                    I("pe", "matmul", self.ps[pc][:, 0:256], self.sel[0:64, r, :], self.maskT[0:64, q0:q0 + 256], start=False, stop=True,
                      reads=["mconst", "maskT"], writes=["ps%d" % pc])
                elif kt == 2 * b:
                    I("pe", "matmul", self.ps[pc][:, 0:256], self.KT[:, hh, k0:k0 + 128], self.QT[:, hh, q0:q0 + 256], start=True, stop=False,
                      reads=["KT%d" % hh, "QT%d" % hh], writes=["ps%d" % pc])
                    I("pe", "matmul", self.ps[pc][:, 0:128], self.identB, self.triB, start=False, stop=True, reads=["mconst"], writes=["ps%d" % pc])
                else:
                    I("pe", "matmul", self.ps[pc][:, 128:256], self.KT[:, hh, k0:k0 + 128], self.QT[:, hh, q0 + 128:q0 + 256], start=True, stop=False,
                      reads=["KT%d" % hh, "QT%d" % hh], writes=["ps%d" % pc])
                    I("pe", "matmul", self.ps[pc][:, 128:256], self.identB, self.triB, start=False, stop=True, reads=["mconst"], writes=["ps%d" % pc])

            def emit_rest(i):
                hh, b, kt = steps[i]
                pc = PCB[i % 4]
                pT = self.pT[i % 4]
                ptag = "pT%d" % (i % 4)
                pO = 2 + b % 2
                pS = 4 + b % 2
                q0 = b * 256
                last = 2 * b + 1
                lo = 128 if kt == last else 0
                I("act", "activation", out=pT[:, lo:256], in_=self.ps[pc][:, lo:256], func=AF.Exp, reads=["ps%d" % pc], writes=[ptag])
                I("pe", "matmul", self.ps[pO][:, lo:256], self.V[:, kt, hh * 128:(hh + 1) * 128], pT[:, lo:256], start=(kt == 0), stop=(kt == last),
                  reads=["V", ptag], writes=["ps%d" % pO])
                I("pe", "matmul", self.ps[pS][:, lo:256], self.onesB, pT[:, lo:256], start=(kt == 0), stop=(kt == last),
                  reads=["mconst", ptag], writes=["ps%d" % pS])
                if kt == last:
                    yo = self.yo[b % 2]
                    I("dve", "reciprocal", out=self.rc, in_=self.ps[pS][:, 0:256], reads=["ps%d" % pS], writes=["rc"])
                    I("dve", "tensor_tensor", out=yo, in0=self.ps[pO][:, 0:256], in1=self.rc, op=ALU.mult, reads=["ps%d" % pO, "rc"], writes=["yo%d" % (b % 2)])
                    hrow = (hg * 4 + hh) * 128
                    c.dma("sp", self.yT[hrow:hrow + 128, q0:q0 + 256], yo, reads=["yo%d" % (b % 2)], writes=["yT"])

            LA = 2
            for i in range(min(LA, len(steps))):
                emit_qk(i)
            for i in range(len(steps)):
                if i + LA < len(steps):
                    emit_qk(i + LA)
                emit_rest(i)


    def m_alloc_rwkv(self):
        A, cfg, c = self.arena, self.cfg, self.c
        A.reset(self.const_words)
        self.rw_w = [A.bf16(8192) for _ in range(3)]
        self.rw_wl = A.bf16(16 * 192)
        self.ut = A.bf16(NCH, 512)
        self.w2s = A.f32(1024)
        self.a2s = A.f32(1024)
        self.g2s = A.f32(1024)
        names = ["mu_r", "mu_k", "mu_v", "omu_r", "omu_k", "omu_v", "w0", "a0", "k_k", "k_a", "omka", "r_k", "gn_g", "gn_b"]
        self.pp = {nm: A.f32(8) for nm in names}
        self.mu_l = A.f32(3)
        self.omu_l = A.f32(3)
        self.carry = A.f32(32)
        self.Tst = [A.f32(8, 64) for _ in range(2)]
        self.tw = A.f32(512)
        self.al = A.f32(512)
        self.sgl = A.f32(512)
        self.zbuf = [A.f32(513) for _ in range(2)]
        for nm in ["R", "K", "V", "LW", "A_", "G", "KK", "T1", "BON", "CUM", "EP", "RW", "YR", "G0T", "H0P"]:
            setattr(self, "b_" + nm, A.f32(512))
        self.tm = A.bf16(4, 4, 128)
        for hs in range(2):
            setattr(self, "h%d_MT" % hs, A.f32(512))
            for nm in ["X0", "X1", "Xt0", "Xt1", "MTb", "Aak", "Arb", "Ark", "T1s", "UW"]:
                setattr(self, "h%d_%s" % (hs, nm), A.bf16(512))
        for nm in ["Rb", "Kb", "Bb", "Ab", "Vb"]:
            setattr(self, "b_" + nm, A.bf16(512))
        self.rw_identB = A.bf16(128)
        c.I("dve", "tensor_copy", out=self.rw_identB, in_=self.ident, reads=["cst"], writes=["rconst"])
        self.maskU4 = A.f32(512)
        self.maskUe4 = A.f32(512)
        self.maskL4 = A.f32(512)
        self.ident4 = A.f32(512)
        self.istack8 = A.f32(512)
        self.yob = A.bf16(512)
        for i in range(4):
            cs = slice(i * 128, (i + 1) * 128)
            c.I("pool", "tensor_copy", out=self.maskU4[:, cs], in_=self.maskU, reads=["cst"], writes=["rconst"])
            c.I("pool", "tensor_copy", out=self.maskUe4[:, cs], in_=self.maskUe, reads=["cst"], writes=["rconst"])
            c.I("pool", "tensor_copy", out=self.maskL4[:, cs], in_=self.maskL, reads=["cst"], writes=["rconst"])
            c.I("pool", "tensor_copy", out=self.ident4[:, cs], in_=self.ident, reads=["cst"], writes=["rconst"])
        c.I("pool", "tensor_tensor", out=self.istack8[:, 0:64], in0=self.ident[:, 0:64], in1=self.ident[:, 64:128], op=ALU.add, reads=["cst"], writes=["rconst"])
        for i in range(1, 8):
            c.I("pool", "tensor_copy", out=self.istack8[:, i * 64:(i + 1) * 64], in_=self.istack8[:, 0:64], reads=["rconst"], writes=["rconst"])

    def m_rwkv_params(self, e):
        c = self.c
        inp = self.inp
        pp = self.pp

        def ld(dst, src1d):
            c.dma("sp", dst, src1d.rearrange("(hp p) -> p hp", p=128), writes=["rparam"])

        mu = inp["rwkv_mu"][e]
        ld(pp["mu_r"], mu[0:1024])
        ld(pp["mu_k"], mu[1024:2048])
        ld(pp["mu_v"], mu[2048:3072])
        c.dma("sp", self.mu_l[0:64, :], mu[3072:3264].rearrange("(q p) -> p q", p=64), writes=["rparam"])
        ld(pp["w0"], inp["rwkv_w0"][e])
        ld(pp["a0"], inp["rwkv_a0"][e])
        ld(pp["k_k"], inp["rwkv_k_k"][e])
        ld(pp["k_a"], inp["rwkv_k_a"][e])
        ld(pp["r_k"], inp["rwkv_r_k"][e].rearrange("h d -> (h d)"))
        ld(pp["gn_g"], inp["rwkv_gn_g"][e])
        ld(pp["gn_b"], inp["rwkv_gn_b"][e])
        c.dma("sp", self.w2s[0:64, :], inp["rwkv_w2"][e], writes=["rparam"])
        c.dma("sp", self.a2s[0:64, :], inp["rwkv_a2"][e], writes=["rparam"])
        c.dma("sp", self.g2s[0:64, :], inp["rwkv_g2"][e], writes=["rparam"])
        for a, b in (("omu_r", "mu_r"), ("omu_k", "mu_k"), ("omu_v", "mu_v"), ("omka", "k_a")):
            c.I("dve", "tensor_scalar", out=pp[a], in0=pp[b], scalar1=-1.0, scalar2=1.0, op0=ALU.mult, op1=ALU.add, reads=["rparam"], writes=["rparam"])
        c.I("dve", "tensor_scalar", out=self.omu_l[0:64, :], in0=self.mu_l[0:64, :], scalar1=-1.0, scalar2=1.0, op0=ALU.mult, op1=ALU.add, reads=["rparam"], writes=["rparam"])

    def _lerp(self, psb, P, cidx, mu, omu, out, otag, first):
        c = self.c
        self._zi = getattr(self, "_zi", 0) + 1
        zi = self._zi % 2
        zb = self.zbuf[zi]
        zt = "zbuf%d" % zi
        ctag = "carry%d" % cidx
        c.I("act", "activation", out=zb[0:P, 1:513], in_=self.ps[psb][0:P, 0:512], func=AF.Copy, reads=["ps%d" % psb], writes=[zt])
        if first:
            c.I("pool", "memset", zb[0:P, 0:1], 0.0, writes=[zt])
        else:
            c.I("pool", "tensor_copy", out=zb[0:P, 0:1], in_=self.carry[0:P, cidx:cidx + 1], reads=[ctag], writes=[zt])
        c.I("pool", "tensor_copy", out=self.carry[0:P, cidx:cidx + 1], in_=zb[0:P, 512:513], reads=[zt], writes=[ctag])
        c.I("dve", "tensor_scalar", out=out[0:P, :], in0=zb[0:P, 0:512], scalar1=mu, scalar2=None, op0=ALU.mult, reads=[zt, "rparam"], writes=[otag])
        c.I("dve", "scalar_tensor_tensor", out=out[0:P, :], in0=zb[0:P, 1:513], scalar=omu, in1=out[0:P, :], op0=ALU.mult, op1=ALU.add,
            reads=[zt, "rparam", otag], writes=[otag])

    def m_rwkv(self, e_idx):
        c, cfg = self.c, self.cfg
        S, NT = cfg.S, cfg.S // 512
        WIN = self.WIN[e_idx]
        uTv = self.uT.rearrange("(c p) s -> p c s", p=128)
        pp = self.pp
        ps = self.ps
        I = c.I
        B = lambda nm: getattr(self, "b_" + nm)
        H = lambda nm: getattr(self, "h_" + nm)
        allut = ["ut%d" % ch for ch in range(NCH)]
        self.m_rwkv_params(e_idx)
        c.dma("sp", self.rw_wl, WIN[12][:, 0:16 * 192], reads=["WIN%d_12" % e_idx], writes=["rw_wl"])
        wl = self.rw_wl.rearrange("p (c f) -> p c f", f=192)
        gchunk = 0
        for pg in range(2):
            for q in range(3):
                c.dma("sp", self.rw_w[q], WIN[6 + 2 * q + pg], reads=["WIN%d_%d" % (e_idx, 6 + 2 * q + pg)], writes=["rw_w%d" % q])
            wq3 = [w.rearrange("p (c f) -> p c f", f=512) for w in self.rw_w]
            for t in range(NT):
                t0 = t * 512
                first = (t == 0)
                c.dma("sp", self.ut, uTv[:, :, t0:t0 + 512], reads=["uT"], writes=allut)
                for q, (dst, dtag, fn) in enumerate(((self.tw, "tw", AF.Tanh), (self.al, "al", AF.Copy), (self.sgl, "sgl", AF.Sigmoid))):
                    for ch in range(NCH):
                        I("pe", "matmul", ps[0][0:64, 0:512], wl[:, ch, q * 64:(q + 1) * 64], self.ut[:, ch, :], start=(ch == 0), stop=(ch == NCH - 1),
                          reads=["rw_wl", "ut%d" % ch], writes=["ps0"])
                    self._lerp(0, 64, 24 + q, self.mu_l[0:64, q:q + 1], self.omu_l[0:64, q:q + 1], dst, dtag, first)
                    if fn != AF.Copy:
                        I("act", "activation", out=dst[0:64, :], in_=dst[0:64, :], func=fn, reads=[dtag], writes=[dtag])
                LVL = int(os.environ.get("RWLVL", "6"))
                for pl in range(4 if LVL >= 2 else 0):
                    hp = pg * 4 + pl
                    col = slice(hp, hp + 1)
                    for q, (nm, mun) in enumerate((("R", "r"), ("K", "k"), ("V", "v"))):
                        pb = q % 2
                        for ch in range(NCH):
                            I("pe", "matmul", ps[pb][:, 0:512], wq3[q][:, ch, pl * 128:(pl + 1) * 128], self.ut[:, ch, :], start=(ch == 0), stop=(ch == NCH - 1),
                              reads=["rw_w%d" % q, "ut%d" % ch], writes=["ps%d" % pb])
                        self._lerp(pb, 128, q * 8 + hp, pp["mu_" + mun][:, col], pp["omu_" + mun][:, col], B(nm), "b_" + nm, first)
                    R, K, V, LW, A_, G, KK, T1, BON, CUM, EP = (B(x) for x in ("R", "K", "V", "LW", "A_", "G", "KK", "T1", "BON", "CUM", "EP"))
                    cs128 = slice(hp * 128, (hp + 1) * 128)
                    I("pe", "matmul", ps[0][:, 0:512], self.w2s[0:64, cs128], self.tw[0:64, :], start=True, stop=True, reads=["rparam", "tw"], writes=["ps0"])
                    I("act", "activation", out=LW, in_=ps[0][:, 0:512], func=AF.Sigmoid, bias=pp["w0"][:, col], scale=1.0, reads=["ps0", "rparam"], writes=["b_LW"])
                    I("pool", "tensor_scalar", out=LW, in0=LW, scalar1=-math.exp(-0.5), scalar2=None, op0=ALU.mult, reads=["b_LW"], writes=["b_LW"])
                    I("pe", "matmul", ps[1][:, 0:512], self.a2s[0:64, cs128], self.al[0:64, :], start=True, stop=True, reads=["rparam", "al"], writes=["ps1"])
                    I("act", "activation", out=A_, in_=ps[1][:, 0:512], func=AF.Sigmoid, bias=pp["a0"][:, col], scale=1.0, reads=["ps1", "rparam"], writes=["b_A_"])
                    I("pe", "matmul", ps[0][:, 0:512], self.g2s[0:64, cs128], self.sgl[0:64, :], start=True, stop=True, reads=["rparam", "sgl"], writes=["ps0"])
                    I("act", "activation", out=G, in_=ps[0][:, 0:512], func=AF.Copy, reads=["ps0"], writes=["b_G"])
                    I("dve", "tensor_scalar", out=KK, in0=K, scalar1=pp["k_k"][:, col], scalar2=None, op0=ALU.mult, reads=["b_K", "rparam"], writes=["b_KK"])
                    I("pool", "tensor_tensor", out=T1, in0=KK, in1=KK, op=ALU.mult, reads=["b_KK"], writes=["b_T1"])
                    I("pe", "matmul", ps[1][:, 0:512], self.bones, T1, start=True, stop=True, reads=["cst", "b_T1"], writes=["ps1"])
                    I("dve", "tensor_scalar", out=T1, in0=ps[1][:, 0:512], scalar1=1e-24, scalar2=None, op0=ALU.max, reads=["ps1"], writes=["b_T1"])
                    I("act", "activation", out=T1, in_=T1, func=AF.Sqrt, reads=["b_T1"], writes=["b_T1"])
                    I("dve", "reciprocal", out=T1, in_=T1, reads=["b_T1"], writes=["b_T1"])
                    I("dve", "tensor_tensor", out=KK, in0=KK, in1=T1, op=ALU.mult, reads=["b_KK", "b_T1"], writes=["b_KK"])
                    I("dve", "tensor_scalar", out=T1, in0=A_, scalar1=pp["k_a"][:, col], scalar2=pp["omka"][:, col], op0=ALU.mult, op1=ALU.add,
                      reads=["b_A_", "rparam"], writes=["b_T1"])
                    I("pool", "tensor_tensor", out=K, in0=K, in1=T1, op=ALU.mult, reads=["b_K", "b_T1"], writes=["b_K"])
                    I("pool", "tensor_tensor", out=A_, in0=KK, in1=A_, op=ALU.mult, reads=["b_KK", "b_A_"], writes=["b_A_"])
                    I("dve", "scalar_tensor_tensor", out=T1, in0=R, scalar=pp["r_k"][:, col], in1=K, op0=ALU.mult, op1=ALU.mult, reads=["b_R", "b_K", "rparam"], writes=["b_T1"])
                    I("pe", "matmul", ps[0][:, 0:512], self.bones, T1, start=True, stop=True, reads=["cst", "b_T1"], writes=["ps0"])
                    I("act", "activation", out=BON, in_=ps[0][:, 0:512], func=AF.Copy, reads=["ps0"], writes=["b_BON"])
                    I("dve", "tensor_tensor_scan", out=CUM, data0=self.reset, data1=LW, initial=0.0, op0=ALU.mult, op1=ALU.add, reads=["cst", "b_LW"], writes=["b_CUM"])
                    I("pool", "tensor_tensor", out=LW, in0=CUM, in1=LW, op=ALU.subtract, reads=["b_CUM", "b_LW"], writes=["b_LW"])
                    I("act", "activation", out=EP, in_=CUM, func=AF.Exp, reads=["b_CUM"], writes=["b_EP"])
                    I("act", "activation", out=CUM, in_=CUM, func=AF.Exp, scale=-1.0, reads=["b_CUM"], writes=["b_CUM"])
                    I("act", "activation", out=LW, in_=LW, func=AF.Exp, reads=["b_LW"], writes=["b_LW"])
                    Rb, Kb, Bb, Ab, Vb = (B(x) for x in ("Rb", "Kb", "Bb", "Ab", "Vb"))
                    I("dve", "tensor_tensor", out=R, in0=R, in1=EP, op=ALU.mult, reads=["b_R", "b_EP"], writes=["b_R"])
                    I("pool", "tensor_copy", out=Rb, in_=R, reads=["b_R"], writes=["b_Rb"])
                    I("pool", "tensor_tensor", out=Kb, in0=K, in1=CUM, op=ALU.mult, reads=["b_K", "b_CUM"], writes=["b_Kb"])
                    I("pool", "tensor_tensor", out=Bb, in0=A_, in1=CUM, op=ALU.mult, reads=["b_A_", "b_CUM"], writes=["b_Bb"])
                    I("dve", "scalar_tensor_tensor", out=Ab, in0=KK, scalar=-1.0, in1=LW, op0=ALU.mult, op1=ALU.mult, reads=["b_KK", "b_LW"], writes=["b_Ab"])
                    I("pool", "tensor_copy", out=Vb, in_=V, reads=["b_V"], writes=["b_Vb"])
                    for cp in range(4 if LVL >= 3 else 0):
                        cs = slice(cp * 128, (cp + 1) * 128)
                        for ty, (src, stag) in enumerate(((Vb, "b_Vb"), (Ab, "b_Ab"), (Bb, "b_Bb"), (Kb, "b_Kb"))):
                            I("pe", "matmul", ps[2][:, ty * 128:(ty + 1) * 128], src[:, cs], self.rw_identB, start=True, stop=True, reads=[stag, "rconst"], writes=["ps2"])
                        eng = "act" if cp % 2 else "dve"
                        if eng == "act":
                            I("act", "activation", out=self.tm[:, cp].rearrange("p a b -> p (a b)"), in_=ps[2][:, 0:512], func=AF.Copy, reads=["ps2"], writes=["tm%d" % cp])
                        else:
                            I("dve", "tensor_copy", out=self.tm[:, cp].rearrange("p a b -> p (a b)"), in_=ps[2][:, 0:512], reads=["ps2"], writes=["tm%d" % cp])
                    tmtags = ["tm%d" % i for i in range(4)]
                    RW, YR, G0T, H0P = B("RW"), B("YR"), B("G0T"), B("H0P")
                    if LVL < 4:
                        continue
                    def head_gen(hs):
                        P_ = slice(64 * hs, 64 * hs + 64)
                        hc = P_
                        ba, bb = (0, 1) if hs == 0 else (2, 3)
                        Hh = lambda nm: getattr(self, "h%d_%s" % (hs, nm))
                        tg = lambda nm: "h%d_%s" % (hs, nm)
                        X = [Hh("X0"), Hh("X1")]
                        Xt = [Hh("Xt0"), Hh("Xt1")]
                        MT, MTb, Aak, Arb, Ark, T1s, UW = Hh("MT"), Hh("MTb"), Hh("Aak"), Hh("Arb"), Hh("Ark"), Hh("T1s"), Hh("UW")
                        CS = [slice(cp * 128, (cp + 1) * 128) for cp in range(4)]

                        def amat(bank, l, ltag, r, rtag):
                            for cs in CS:
                                I("pe", "matmul", ps[bank][:, cs], l[P_, cs], r[P_, cs], start=True, stop=True, reads=[ltag, rtag], writes=["ps%d" % bank])

                        amat(ba, Ab, "b_Ab", Bb, "b_Bb")
                        amat(bb, Bb, "b_Bb", Ab, "b_Ab")
                        yield
                        I("dve", "tensor_tensor", out=X[0], in0=ps[ba][:, 0:512], in1=self.maskL4, op=ALU.mult, reads=["ps%d" % ba, "rconst"], writes=[tg("X0")])
                        I("dve", "tensor_tensor", out=Xt[0], in0=ps[bb][:, 0:512], in1=self.maskU4, op=ALU.mult, reads=["ps%d" % bb, "rconst"], writes=[tg("Xt0")])
                        amat(ba, Kb, "b_Kb", Ab, "b_Ab")
                        amat(bb, Bb, "b_Bb", Rb, "b_Rb")
                        yield
                        I("pool", "tensor_tensor", out=MT, in0=Xt[0], in1=self.ident4, op=ALU.add, reads=[tg("Xt0"), "rconst"], writes=[tg("MT")])
                        I("pool", "tensor_copy", out=MTb, in_=MT, reads=[tg("MT")], writes=[tg("MTb")])
                        I("dve", "tensor_tensor", out=Aak, in0=ps[ba][:, 0:512], in1=self.maskU4, op=ALU.mult, reads=["ps%d" % ba, "rconst"], writes=[tg("Aak")])
                        I("dve", "tensor_tensor", out=Arb, in0=ps[bb][:, 0:512], in1=self.maskUe4, op=ALU.mult, reads=["ps%d" % bb, "rconst"], writes=[tg("Arb")])
                        amat(ba, Kb, "b_Kb", Rb, "b_Rb")
                        yield
                        I("dve", "tensor_tensor", out=Ark, in0=ps[ba][:, 0:512], in1=self.maskUe4, op=ALU.mult, reads=["ps%d" % ba, "rconst"], writes=[tg("Ark")])
                        cur = 0
                        for k in range(5):
                            nx = 1 - cur
                            for cs in CS:
                                I("pe", "matmul", ps[bb][:, cs], Xt[cur][:, cs], X[cur][:, cs], start=True, stop=True, reads=[tg("X%d" % cur), tg("Xt%d" % cur)], writes=["ps%d" % bb])
                            if k < 4:
                                for cs in CS:
                                    I("pe", "matmul", ps[ba][:, cs], X[cur][:, cs], Xt[cur][:, cs], start=True, stop=True, reads=[tg("X%d" % cur), tg("Xt%d" % cur)], writes=["ps%d" % ba])
                            yield
                            I("act", "activation", out=X[nx], in_=ps[bb][:, 0:512], func=AF.Copy, reads=["ps%d" % bb], writes=[tg("X%d" % nx)])
                            if k < 4:
                                I("dve", "tensor_copy", out=Xt[nx], in_=ps[ba][:, 0:512], reads=["ps%d" % ba], writes=[tg("Xt%d" % nx)])
                            for cs in CS:
                                I("pe", "matmul", ps[bb][:, cs], X[nx][:, cs], MTb[:, cs], start=True, stop=True, reads=[tg("X%d" % nx), tg("MTb")], writes=["ps%d" % bb])
                            yield
                            I("dve", "tensor_tensor", out=MT, in0=ps[bb][:, 0:512], in1=MT, op=ALU.add, reads=["ps%d" % bb, tg("MT")], writes=[tg("MT")])
                            I("pool", "tensor_copy", out=MTb, in_=MT, reads=[tg("MT")], writes=[tg("MTb")])
                            cur = nx
                        for cp in range(4):
                            I("pe", "matmul", ps[ba][:, cp * 64:(cp + 1) * 64], Aak[:, CS[cp]], self.tm[:, cp, 0, hc], start=True, stop=True, reads=[tg("Aak"), "tm%d" % cp], writes=["ps%d" % ba])
                        yield
                        I("act", "activation", out=T1s[:, 0:256], in_=ps[ba][:, 0:256], func=AF.Copy, reads=["ps%d" % ba], writes=[tg("T1s")])
                        for cp in range(4):
                            I("pe", "matmul", ps[bb][:, cp * 128:cp * 128 + 64], MTb[:, CS[cp]], T1s[:, cp * 64:(cp + 1) * 64], start=True, stop=True, reads=[tg("MTb"), tg("T1s")], writes=["ps%d" % bb])
                            I("pe", "matmul", ps[bb][:, cp * 128 + 64:cp * 128 + 128], MTb[:, CS[cp]], self.tm[:, cp, 1, hc], start=True, stop=True, reads=[tg("MTb"), "tm%d" % cp], writes=["ps%d" % bb])
                        yield
                        I("dve", "tensor_copy", out=UW, in_=ps[bb][:, 0:512], reads=["ps%d" % bb], writes=[tg("UW")])
                        for cp in range(4):
                            I("pe", "matmul", ps[ba][P_, CS[cp]], UW[:, cp * 128 + 64:cp * 128 + 128], Arb[:, CS[cp]], start=True, stop=True, reads=[tg("UW"), tg("Arb")], writes=["ps%d" % ba])
                        for cp in range(4):
                            I("pe", "matmul", ps[5][P_, CS[cp]], UW[:, cp * 128:cp * 128 + 64], Arb[:, CS[cp]], start=(cp == 0), stop=False, skip_group_check=True,
                              reads=[tg("UW"), tg("Arb")], writes=["ps5"])
                            I("pe", "matmul", ps[5][P_, CS[cp]], self.tm[:, cp, 0, hc], Ark[:, CS[cp]], start=False, stop=False, skip_group_check=True,
                              reads=["tm%d" % cp, tg("Ark")], writes=["ps5"])
                        yield
                        I("dve", "tensor_tensor", out=RW[P_, :], in0=ps[ba][P_, 0:512], in1=R[P_, :], op=ALU.add, reads=["ps%d" % ba, "b_R"], writes=["b_RW%d" % hs])

                    gens = [head_gen(0), head_gen(1)]
                    alive = [True, True]
                    while any(alive):
                        for gi in range(2):
                            if alive[gi]:
                                try:
                                    next(gens[gi])
                                except StopIteration:
                                    alive[gi] = False
                    for c2 in range(2):
                        for hs in range(2):
                            P_ = slice(64 * hs, 64 * hs + 64)
                            hc = P_
                            UW = getattr(self, "h%d_UW" % hs)
                            uwt = "h%d_UW" % hs
                            for cp in range(4):
                                qq = cp * 2 + c2
                                TP = slice(64 * c2, 64 * c2 + 64)
                                qs = slice(qq * 64, (qq + 1) * 64)
                                sw = (cp == 0 and hs == 0)
                                I("pe", "matmul", ps[6][P_, qs], UW[TP, cp * 128 + 64:cp * 128 + 128], self.tm[TP, cp, 2, hc], start=True, stop=True, reads=[uwt, "tm%d" % cp], writes=["ps6"], ses=sw)
                                I("pe", "matmul", ps[7][P_, qs], self.tm[TP, cp, 2, hc], UW[TP, cp * 128:cp * 128 + 64], start=(qq == 0), stop=False, skip_group_check=True,
                                  reads=[uwt, "tm%d" % cp], writes=["ps7"], ses=sw)
                                I("pe", "matmul", ps[7][P_, qs], self.tm[TP, cp, 3, hc], self.tm[TP, cp, 0, hc], start=False, stop=True, skip_group_check=True,
                                  reads=["tm%d" % cp], writes=["ps7"])
                    if LVL < 5:
                        continue
                    I("dve", "tensor_tensor", out=G0T, in0=ps[6][:, 0:512], in1=self.istack8, op=ALU.add, reads=["ps6", "rconst"], writes=["b_G0T"])
                    pc8 = EP.rearrange("p (q c) -> p q c", c=64)[:, :, 63:64].to_broadcast([128, 8, 64])
                    I("dve", "tensor_tensor", out=H0P.rearrange("p (q c) -> p q c", c=64), in0=ps[7][:, 0:512].rearrange("p (q c) -> p q c", c=64), in1=pc8, op=ALU.mult,
                      reads=["ps7", "b_EP"], writes=["b_H0P"])
                    for qq in range(8):
                        qs = slice(qq * 64, (qq + 1) * 64)
                        Tc = self.Tst[gchunk % 2]
                        Tn = self.Tst[(gchunk + 1) % 2]
                        tct = "Tst%d_%d" % (gchunk % 2, hp)
                        tnt = "Tst%d_%d" % ((gchunk + 1) % 2, hp)
                        if first and qq == 0:
                            I("pool", "memset", Tc[:, hp, :], 0.0, writes=[tct])
                        for hs in range(2):
                            P_ = slice(64 * hs, 64 * hs + 64)
                            I("pe", "matmul", ps[5][P_, qs], Tc[P_, hp, :], RW[P_, qs], start=False, stop=True, skip_group_check=True, reads=[tct, "b_RW%d" % hs], writes=["ps5"],
                              ses=True)
                            I("pe", "matmul", ps[0][P_, qs], G0T[P_, qs], Tc[P_, hp, :], start=True, stop=True, reads=[tct, "b_G0T"], writes=["ps0"], ses=True)
                        I("dve", "scalar_tensor_tensor", out=Tn[:, hp, :], in0=ps[0][:, qs], scalar=EP[:, qq * 64 + 63:qq * 64 + 64], in1=H0P[:, qs], op0=ALU.mult, op1=ALU.add,
                          reads=["ps0", "b_EP", "b_H0P"], writes=[tnt])
                        gchunk += 1
                    if LVL < 6:
                        continue
                    I("act", "activation", out=YR, in_=ps[5][:, 0:512], func=AF.Copy, reads=["ps5"], writes=["b_YR"])
                    I("pe", "matmul", ps[1][:, 0:512], self.bones, YR, start=True, stop=True, reads=["cst", "b_YR"], writes=["ps1"])
                    I("dve", "scalar_tensor_tensor", out=YR, in0=ps[1][:, 0:512], scalar=-1.0 / 64, in1=YR, op0=ALU.mult, op1=ALU.add, reads=["ps1", "b_YR"], writes=["b_YR"])
                    I("pool", "tensor_tensor", out=T1, in0=YR, in1=YR, op=ALU.mult, reads=["b_YR"], writes=["b_T1"])
                    I("pe", "matmul", ps[1][:, 0:512], self.bones, T1, start=True, stop=True, reads=["cst", "b_T1"], writes=["ps1"])
                    I("act", "activation", out=T1, in_=ps[1][:, 0:512], func=AF.Sqrt, bias=self.epsc[:, 1:2], scale=1.0 / 64, reads=["ps1", "cst"], writes=["b_T1"])
                    I("dve", "reciprocal", out=T1, in_=T1, reads=["b_T1"], writes=["b_T1"])
                    I("dve", "tensor_tensor", out=YR, in0=YR, in1=T1, op=ALU.mult, reads=["b_YR", "b_T1"], writes=["b_YR"])
                    I("dve", "tensor_scalar", out=YR, in0=YR, scalar1=pp["gn_g"][:, col], scalar2=pp["gn_b"][:, col], op0=ALU.mult, op1=ALU.add, reads=["b_YR", "rparam"], writes=["b_YR"])
                    I("pool", "tensor_tensor", out=T1, in0=BON, in1=V, op=ALU.mult, reads=["b_BON", "b_V"], writes=["b_T1"])
                    I("pool", "tensor_tensor", out=YR, in0=YR, in1=T1, op=ALU.add, reads=["b_YR", "b_T1"], writes=["b_YR"])
                    I("dve", "tensor_tensor", out=self.yob, in0=YR, in1=G, op=ALU.mult, reads=["b_YR", "b_G"], writes=["yob"])
                    c.dma("sp", self.yT[1024 + hp * 128:1024 + (hp + 1) * 128, t0:t0 + 512], self.yob, reads=["yob"], writes=["yT"])

    def dbg_dump_y(self, r0, nrows):
        c, cfg = self.c, self.cfg
        A = self.arena
        A.reset(self.const_words)
        tb = A.bf16(cfg.S)
        tf = A.f32(cfg.S)
        evs = []
        for r in range(r0, r0 + nrows, 128):
            c.dma("sp", tb, self.yT[r:r + 128, :], reads=["yT"], writes=["dbgb"])
            c.op("dve", lambda e: e.tensor_copy(out=tf, in_=tb), reads=["dbgb"], writes=["dbgf"])
            evs.append(c.dma("sp", self.outT[r:r + 128, :], tf, reads=["dbgf"], writes=["outd"]))
        return evs

    def t_load(self, src, t0, n):
        v = src.rearrange("(c p) s -> p c s", p=128)[:, :, t0:t0 + n]
        self.c.dma("sp", self.hs[:, :, 0:n], v, reads=["hTd"], writes=["hs%d" % ch for ch in range(NCH)])

    def t_store_h(self, t0, n):
        v = self.hT.rearrange("(c p) s -> p c s", p=128)[:, :, t0:t0 + n]
        self.c.dma("sp", v, self.hs[:, :, 0:n], reads=["hs%d" % ch for ch in range(NCH)], writes=["hTd"])

    def t_store_u(self, l, t0, n):
        self.t_norm_stats(n)
        self.t_norm_apply(n, self.gidx[("mix", l)], self.ub, "ub")
        v = self.uT.rearrange("(c p) s -> p c s", p=128)[:, :, t0:t0 + n]
        self.c.dma("sp", v, self.ub[:, :, 0:n], reads=["ub%d" % ch for ch in range(NCH)], writes=["uT"])

    def t_final(self, t0, n):
        c = self.c
        self.t_norm_stats(n)
        gidx = self.gidx[("final", 0)]
        for ch in range(NCH):
            eng = "dve"
            c.op(eng, lambda e, ch=ch: e.scalar_tensor_tensor(out=self.hs[:, ch, 0:n], in0=self.hs[:, ch, 0:n], scalar=self.gains[:, gidx, ch:ch + 1],
                                                            in1=self.rstd[:, 0:n], op0=ALU.mult, op1=ALU.mult),
                 reads=["hs%d" % ch, "rstd"], writes=["hs%d" % ch])
        v = self.outT.rearrange("(c p) s -> p c s", p=128)[:, :, t0:t0 + n]
        return self.c.dma("sp", v, self.hs[:, :, 0:n], reads=["hs%d" % ch for ch in range(NCH)], writes=["outd"])

    def load_consts(self):
        c, cfg, A = self.c, self.cfg, self.arena
        A.reset(0)
        ncw = CONSTS.shape[1]
        self.cst = A.f32(ncw)
        c.dma("sp", self.cst, self.consts, writes=["cst"])

        def cv(name):
            o, w = COFF[name]
            return self.cst[:, o:o + w]

        self.ident = cv("ident")
        self.tri_f = cv("tri")
        self.bones = cv("bones")
        self.maskU = cv("maskU")
        self.maskUe = cv("maskUe")
        self.maskL = cv("maskL")
        self.reset = cv("reset")
        self.invcnt = cv("invcnt").rearrange("p (g t) -> p g t", t=16)
        names = []
        for l in range(cfg.depth):
            names += [("ffn1", l), ("mix", l), ("ffn2", l)]
        names.append(("final", 0))
        self.gidx = {k: i for i, k in enumerate(names)}
        self.gains = A.f32(len(names), NCH)
        for (k, l), i in self.gidx.items():
            src = self.inp["final_norm"] if k == "final" else self.inp["%s_norm" % k][l]
            c.dma(self.dq(), self.gains[:, i, :], src.rearrange("(c p) -> p c", p=128), writes=["cst"])
        self.pscale = A.f32(max(cfg.n_odd, 1), NCH)
        for o in range(cfg.n_odd):
            c.dma(self.dq(), self.pscale[:, o, :], self.inp["pool_scale"][o].rearrange("(c p) -> p c", p=128), writes=["cst"])
        self.onesD = A.f32(128)
        c.op("pool", lambda e: e.memset(self.onesD, 1.0 / D), writes=["cst"])
        self.epsc = A.f32(4)
        c.op("pool", lambda e: e.memset(self.epsc[:, 0:1], 1e-6), writes=["cst"])
        c.op("pool", lambda e: e.memset(self.epsc[:, 1:2], 64e-5), writes=["cst"])
        c.op("pool", lambda e: e.memset(self.epsc[:, 2:3], 0.0), writes=["cst"])
        self.const_words = A.off

    def build(self):
        nc, cfg = self.nc, self.cfg
        self.declare()
        with self.es:
            NW = 50 * 1024
            arena_t = self.es.enter_context(nc.sbuf_tensor("arena", [128, NW], F32))
            self.arena = Arena(arena_t, NW)
            self.ps = [self.es.enter_context(nc.psum_tensor("psb%d" % i, [128, 512], F32)) for i in range(8)]
            self.c = Ctx(nc)
            with nc.allow_non_contiguous_dma("small parameter vectors"):
                self.load_consts()
                final = self.program()
                self.c.finish(final)
        return nc

    def program(self):
        c, cfg = self.c, self.cfg
        st = cfg.stages
        self.prep()
        self.t_alloc()
        final = []
        TT = cfg.TT
        if st == "ffn":
            for t in range(cfg.NT):
                self.t_load(self.xT, t * TT, TT)
                self.t_ffn(TT, 0, 1)
                final.append(self.t_final(t * TT, TT))
            return final
        if st == "pool":
            for t in range(cfg.NT):
                self.t_load(self.xT, t * TT, TT)
                self.t_ffn(TT, 1, 1)
                self.t_pool(TT, 1, t == 0)
                self.t_ffn(TT, 1, 2)
                final.append(self.t_final(t * TT, TT))
            return final
        if st == "moba":
            for t in range(cfg.NT):
                self.t_load(self.xT, t * TT, TT)
                self.t_store_u(0, t * TT, TT)
            c.barrier()
            self.m_alloc_moba()
            self.m_moba(0)
            c.barrier()
            return self.dbg_dump_y(0, 1024)
        if st == "rwkv":
            for t in range(cfg.NT):
                self.t_load(self.xT, t * TT, TT)
                self.t_store_u(0, t * TT, TT)
            c.barrier()
            self.m_alloc_rwkv()
            self.m_rwkv(0)
            c.barrier()
            return self.dbg_dump_y(1024, 1024)
        assert cfg.depth == 4
        NT = cfg.NT

        def mixer(e_idx):
            c.barrier()
            self.m_alloc_moba()
            self.m_moba(e_idx)
            c.barrier()
            self.m_alloc_rwkv()
            self.m_rwkv(e_idx)
            c.barrier()
            self.t_alloc()

        for t in range(NT):
            self.t_load(self.xT, t * TT, TT)
            self.t_ffn(TT, 0, 1)
            self.t_store_u(0, t * TT, TT)
            self.t_store_h(t * TT, TT)
        mixer(0)
        for t in range(NT):
            self.t_load(self.hT, t * TT, TT)
            self.t_wout(TT, 0, t * TT)
            self.t_ffn(TT, 0, 2)
            self.t_ffn(TT, 1, 1)
            self.t_pool(TT, 1, t == 0)
            self.t_ffn(TT, 1, 2)
            self.t_ffn(TT, 2, 1)
            self.t_store_u(2, t * TT, TT)
            self.t_store_h(t * TT, TT)
        mixer(1)
        for t in range(NT):
            self.t_load(self.hT, t * TT, TT)
            self.t_wout(TT, 1, t * TT)
            self.t_ffn(TT, 2, 2)
            self.t_ffn(TT, 3, 1)
            self.t_pool(TT, 3, t == 0)
            self.t_ffn(TT, 3, 2)
            final.append(self.t_final(t * TT, TT))
        return final


_CACHE = {}

IN_NAMES = ["ffn1_norm", "ffn1_wg", "ffn1_wu", "ffn1_wd", "mix_norm", "ffn2_norm", "ffn2_wg", "ffn2_wu", "ffn2_wd",
            "ab_w_in", "ab_w_out", "rwkv_mu", "rwkv_w0", "rwkv_w2", "rwkv_a0", "rwkv_a2", "rwkv_g2", "rwkv_k_k", "rwkv_k_a",
            "rwkv_r_k", "rwkv_gn_g", "rwkv_gn_b", "pool_w", "pool_scale", "final_norm"]


def kernel(**inputs):
    x = np.asarray(inputs["x"], dtype=np.float32)
    Bn, S, Dm = x.shape
    assert Dm == D and Bn == 4
    dff = int(np.asarray(inputs["ffn1_wg"]).shape[2])
    depth = int(np.asarray(inputs["ffn1_wg"]).shape[0])
    key = (S, depth, dff)
    if key not in _CACHE:
        _CACHE[key] = Builder(Cfg(S=S, depth=depth, dff=dff)).build()
    nc = _CACHE[key]
    shared = {n: np.ascontiguousarray(np.asarray(inputs[n], dtype=np.float32)) for n in IN_NAMES}
    shared["consts"] = CONSTS
    xT = [np.ascontiguousarray(x[b].T) for b in range(Bn)]
    owner = [0, 1, 4, 5]
    zeros = np.zeros_like(xT[0])
    in_maps = []
    for core in range(8):
        m = dict(shared)
        m["xT"] = xT[owner.index(core)] if core in owner else zeros
        in_maps.append(m)
    res = run_bass_kernel_spmd(nc, in_maps, core_ids=list(range(8)))
    out = np.stack([np.ascontiguousarray(res.results[owner[b]]["outT"].T) for b in range(Bn)], axis=0)
    return out.astype(np.float32)
```

```python
import contextlib
import math
import os
import numpy as np
import concourse.bass as bass
import concourse.mybir as mybir
from concourse.bass_utils import run_bass_kernel_spmd

F32 = mybir.dt.float32
BF16 = mybir.dt.bfloat16
ALU = mybir.AluOpType
AF = mybir.ActivationFunctionType
AX = mybir.AxisListType

EPOCH = 30000
D = 2048
NCH = 16
NEG = -30000.0
import os
DBG = {k: 1 for k in os.environ.get('KDBG', '').split(',') if k}


class Tag:
    __slots__ = ("name", "last_w", "readers")

    def __init__(self, name):
        self.name = name
        self.last_w = None
        self.readers = []


class Ctx:
    def __init__(self, nc, n_dma_sems=32):
        self.nc = nc
        self.stack = contextlib.ExitStack()
        self.eng_names = ["pe", "dve", "act", "pool", "sp"]
        self.prog = {e: [] for e in self.eng_names}
        self.cnt = {e: 0 for e in self.eng_names}
        self.sem = {}
        self.semid = 0
        for e in self.eng_names:
            self.sem[e] = self._new_sem("e_" + e)
        self.waited = {e: {} for e in self.eng_names}
        self.n_main_sems = n_dma_sems
        self.n_pool_sems = int(os.environ.get("KPOOLSEMS", "8"))
        self.dma_sems = [self._new_sem("d%d" % i) for i in range(n_dma_sems + self.n_pool_sems)]
        self.dma_val = [0] * (n_dma_sems + self.n_pool_sems)
        self.dma_rr = 0
        self.pool_rr = 0
        self.tags = {}
        self.same_engine_sync = {"pe": False, "dve": True, "act": True, "pool": True, "sp": False}
        self.n_instr = 0
        self.last_ev = {}

    def _new_sem(self, name):
        self.semid += 1
        return self.stack.enter_context(self.nc.semaphore("%s_%d" % (name, self.semid)))

    def tag(self, name):
        t = self.tags.get(name)
        if t is None:
            t = Tag(name)
            self.tags[name] = t
        return t

    def _tags(self, lst):
        return [x if isinstance(x, Tag) else self.tag(x) for x in lst]

    def _deps(self, e, R, W, ses=False):
        deps = []
        for t in R:
            if t.last_w is not None:
                deps.append(t.last_w)
        for t in W:
            if t.last_w is not None:
                deps.append(t.last_w)
            deps.extend(t.readers)
        best = {}
        for (s, v, src) in deps:
            if src == e and not (self.same_engine_sync[e] or ses):
                continue
            k = id(s)
            if k not in best or best[k][1] < v:
                best[k] = (s, v)
        waits = []
        for k, (s, v) in best.items():
            if self.waited[e].get(k, 0) >= v:
                continue
            self.waited[e][k] = v
            waits.append((s, v))
        return waits

    def _commit(self, ev, R, W):
        for t in W:
            t.last_w = ev
            t.readers = []
        for t in R:
            if t not in W:
                t.readers.append(ev)
                if len(t.readers) > 64:
                    best = {}
                    for (s, v, src) in t.readers:
                        k = id(s)
                        if k not in best or best[k][1] < v:
                            best[k] = (s, v, src)
                    t.readers = list(best.values())
        self.n_instr += 1

    def op(self, e, fn, reads=(), writes=(), ses=False):
        pr = [x for x in reads if isinstance(x, str) and x.startswith("ps") and x[2:].isdigit()]
        if pr:
            reads = [x for x in reads if x not in pr]
            writes = list(writes) + [x for x in pr if x not in writes]
        R = self._tags(reads)
        W = self._tags(writes)
        waits = self._deps(e, R, W, ses)
        if self.cnt[e] >= EPOCH:
            self.sem[e] = self._new_sem("e_" + e)
            self.cnt[e] = 0
        self.cnt[e] += 1
        s = self.sem[e]
        ev = (s, self.cnt[e], e)
        self.prog[e].append((waits, fn, (s, 1)))
        self.last_ev[e] = ev
        self._commit(ev, R, W)
        return ev

    def I(self, e, method, *args, reads=(), writes=(), ses=False, **kw):
        return self.op(e, lambda eng: getattr(eng, method)(*args, **kw), reads=reads, writes=writes, ses=ses)

    def dma(self, q, out, in_, reads=(), writes=()):
        R = self._tags(reads)
        W = self._tags(writes)
        waits = self._deps(q, R, W)
        if q == "pool":
            i = self.n_main_sems + self.pool_rr
            self.pool_rr = (self.pool_rr + 1) % self.n_pool_sems
        else:
            i = self.dma_rr
            self.dma_rr = (self.dma_rr + 1) % self.n_main_sems
        s = self.dma_sems[i]
        if self.dma_val[i] > 0 and self.waited[q].get(id(s), 0) < self.dma_val[i]:
            waits.append((s, self.dma_val[i]))
            self.waited[q][id(s)] = self.dma_val[i]
        self.dma_val[i] += 16
        ev = (s, self.dma_val[i], "dma")

        def fn(eng, out=out, in_=in_):
            return eng.dma_start(out=out, in_=in_)

        self.prog[q].append((waits, fn, (s, 16)))
        self._commit(ev, R, W)
        return ev

    def barrier(self):
        evs = [(s, v) for (s, v, _e) in self.last_ev.values()]
        for i, s in enumerate(self.dma_sems[:self.n_main_sems]):
            if self.dma_val[i] > 0:
                evs.append((s, self.dma_val[i]))
        for e in self.eng_names:
            waits = []
            for (s, v) in evs:
                if s is self.sem[e] and not self.same_engine_sync[e]:
                    continue
                if self.waited[e].get(id(s), 0) >= v:
                    continue
                self.waited[e][id(s)] = v
                waits.append((s, v))
            if waits:
                self.prog[e].append((waits, None, None))

    def finish(self, final_events):
        nc = self.nc
        for (s, v, _src) in final_events:
            self.prog["sp"].append(([(s, v)], None, None))
        engmap = {"pe": "tensor", "dve": "vector", "act": "scalar", "pool": "gpsimd", "sp": "sync"}
        with nc.Block() as block:
            for e in self.eng_names:
                plist = self.prog[e]

                def body(eng, plist=plist):
                    for (waits, fn, inc) in plist:
                        for (s, v) in waits:
                            eng.wait_ge(s, v)
                        if fn is not None:
                            fn(eng).then_inc(inc[0], inc[1])

                getattr(block, engmap[e])(body)
        self.stack.close()


class Arena:
    def __init__(self, t, nwords):
        self.t = t
        self.nwords = nwords
        self.off = 0

    def reset(self, off=0):
        self.off = off

    def f32(self, *free):
        n = int(np.prod(free))
        assert self.off + n <= self.nwords, ("arena overflow", self.off, n, self.nwords)
        ap = self.t[:, self.off:self.off + n]
        self.off += n
        return _shape(ap, free)

    def bf16(self, *free):
        n = int(np.prod(free))
        n4 = (n + 1) // 2
        assert self.off + n4 <= self.nwords, ("arena overflow", self.off, n4, self.nwords)
        ap = self.t[:, self.off:self.off + n4].bitcast(BF16)
        self.off += n4
        if n4 * 2 != n:
            ap = ap[:, 0:n]
        return _shape(ap, free)


def _shape(ap, free):
    if len(free) == 1:
        return ap
    if len(free) == 2:
        return ap.rearrange("p (a b) -> p a b", b=free[1])
    if len(free) == 3:
        return ap.rearrange("p (a b c) -> p a b c", b=free[1], c=free[2])
    raise ValueError(free)


def make_consts():
    c = {}
    c["ident"] = np.eye(128, dtype=np.float32)
    k = np.arange(128)[:, None]
    q = np.arange(128)[None, :]
    c["tri"] = np.where(k <= q, 0.0, NEG).astype(np.float32)
    same = (k // 64) == (q // 64)
    c["bones"] = same.astype(np.float32)
    c["maskU"] = (same & (k < q)).astype(np.float32)
    c["maskUe"] = (same & (k <= q)).astype(np.float32)
    c["maskL"] = (same & (k > q)).astype(np.float32)
    reset = np.ones((128, 512), np.float32)
    reset[:, ::64] = 0.0
    c["reset"] = reset
    inv = np.zeros((128, 4, 16), np.float32)
    for gi, w in enumerate((2, 4, 8, 16)):
        inv[:, gi, :] = 1.0 / np.minimum(np.arange(16) + 1, w)
    c["invcnt"] = inv.reshape(128, 64)
    order = ["ident", "tri", "bones", "maskU", "maskUe", "maskL", "reset", "invcnt"]
    offs = {}
    o = 0
    for n in order:
        offs[n] = (o, c[n].shape[1])
        o += c[n].shape[1]
    return np.concatenate([c[n] for n in order], axis=1), offs


CONSTS, COFF = make_consts()


class Cfg:
    def __init__(self, S=4096, depth=4, dff=5632, TT=512, stages=None):
        self.S = S
        self.depth = depth
        self.dff = dff
        self.TT = TT
        self.NT = S // TT
        self.NJ = dff // 128
        self.JB = dff // 512
        self.JG = 4
        self.JQ = self.NJ // self.JG
        self.n_even = (depth + 1) // 2
        self.n_odd = depth // 2
        self.stages = stages


class Builder:
    def __init__(self, cfg):
        self.cfg = cfg
        self.nc = bass.Bass("TRN2", target_bir_lowering=False)
        self.es = contextlib.ExitStack()

    def declare(self):
        nc, cfg = self.nc, self.cfg
        S, dp, dff = cfg.S, cfg.depth, cfg.dff
        ne, no = cfg.n_even, max(cfg.n_odd, 1)

        def din(name, shape):
            return nc.dram_tensor(name, list(shape), F32, kind="ExternalInput").ap()

        self.xT = din("xT", [D, S])
        self.inp = {}
        for n, shp in (("ffn1_norm", [dp, D]), ("ffn1_wg", [dp, D, dff]), ("ffn1_wu", [dp, D, dff]),
                       ("ffn1_wd", [dp, dff, D]), ("mix_norm", [dp, D]), ("ffn2_norm", [dp, D]),
                       ("ffn2_wg", [dp, D, dff]), ("ffn2_wu", [dp, D, dff]), ("ffn2_wd", [dp, dff, D]),
                       ("ab_w_in", [ne, D, 6336]), ("ab_w_out", [ne, D, D]), ("rwkv_mu", [ne, 3264]),
                       ("rwkv_w0", [ne, 1024]), ("rwkv_w2", [ne, 64, 1024]), ("rwkv_a0", [ne, 1024]),
                       ("rwkv_a2", [ne, 64, 1024]), ("rwkv_g2", [ne, 64, 1024]), ("rwkv_k_k", [ne, 1024]),
                       ("rwkv_k_a", [ne, 1024]), ("rwkv_r_k", [ne, 16, 64]), ("rwkv_gn_g", [ne, 1024]),
                       ("rwkv_gn_b", [ne, 1024]), ("pool_w", [no, 4, 512, 512]), ("pool_scale", [no, D]),
                       ("final_norm", [D])):
            self.inp[n] = din(n, shp)
        self.consts = din("consts", list(CONSTS.shape))
        self.outT = nc.dram_tensor("outT", [D, S], F32, kind="ExternalOutput").ap()

        def dint(name, shape, dt):
            return nc.dram_tensor(name, list(shape), dt, kind="Internal").ap()

        self.hT = dint("hT", [D, S], F32)
        self.uT = dint("uT", [D, S], BF16)
        self.yT = dint("yT", [D, S], BF16)
        self.WG = {}
        self.WU = {}
        self.WD = {}
        for l in range(dp):
            for s in (1, 2):
                self.WG[(l, s)] = dint("WG_%d_%d" % (l, s), [cfg.JB, 128, 16 * 512], BF16)
                self.WU[(l, s)] = dint("WU_%d_%d" % (l, s), [cfg.JB, 128, 16 * 512], BF16)
                self.WD[(l, s)] = dint("WD_%d_%d" % (l, s), [4, cfg.JG, 128, cfg.JQ * 512], BF16)
        self.WIN = [dint("WIN_%d" % e, [13, 128, 16 * 512], BF16) for e in range(ne)]
        self.WOUT = [dint("WOUT_%d" % e, [4, 128, 16 * 512], BF16) for e in range(ne)]
        self.PW = [dint("PW_%d" % o, [128, 16 * 512], BF16) for o in range(cfg.n_odd)]

    def ew(self):
        self._ew = getattr(self, "_ew", 0) + 1
        return "dve" if self._ew % 2 else "pool"

    def dq(self):
        return "sp"

    def prep(self):
        c, cfg = self.c, self.cfg

        def conv(src, dst, tag):
            if len(src.shape) == 3:
                dst = dst.rearrange("p (a b) -> p a b", b=src.shape[2])
            c.dma("pool", dst, src, writes=[tag])

        def ffn(l, s_):
            wg = self.inp["ffn%d_wg" % s_][l].rearrange("(c p) f -> p c f", p=128)
            wu = self.inp["ffn%d_wu" % s_][l].rearrange("(c p) f -> p c f", p=128)
            for jb in range(cfg.JB):
                conv(wg[:, :, jb * 512:(jb + 1) * 512], self.WG[(l, s_)][jb], "WG%d%d_%d" % (l, s_, jb))
                conv(wu[:, :, jb * 512:(jb + 1) * 512], self.WU[(l, s_)][jb], "WU%d%d_%d" % (l, s_, jb))
            wd = self.inp["ffn%d_wd" % s_][l].rearrange("(jg jq p) (mb d) -> mb jg p jq d", jq=cfg.JQ, p=128, d=512)
            for mb in range(4):
                for jg in range(cfg.JG):
                    conv(wd[mb, jg], self.WD[(l, s_)][mb, jg], "WD%d%d_%d_%d" % (l, s_, mb, jg))

        def mix(e):
            win = self.inp["ab_w_in"][e].rearrange("(c p) n -> p c n", p=128)
            for cb in range(12):
                conv(win[:, :, cb * 512:(cb + 1) * 512], self.WIN[e][cb], "WIN%d_%d" % (e, cb))
            conv(win[:, :, 6144:6336], self.WIN[e][12][:, 0:16 * 192], "WIN%d_12" % e)
            wo = self.inp["ab_w_out"][e].rearrange("(c p) n -> p c n", p=128)
            for mb in range(4):
                conv(wo[:, :, mb * 512:(mb + 1) * 512], self.WOUT[e][mb], "WOUT%d_%d" % (e, mb))

        def pool(o):
            pw = self.inp["pool_w"][o].rearrange("g (ci p) co -> p g ci co", p=128)
            for g in range(4):
                conv(pw[:, g], self.PW[o][:, g * 2048:(g + 1) * 2048], "PW%d" % o)

        if cfg.depth >= 1:
            ffn(0, 1)
        if cfg.n_even >= 1:
            mix(0)
        for l in range(cfg.depth):
            if l > 0:
                ffn(l, 1)
                if l % 2 == 0 and l // 2 < cfg.n_even:
                    mix(l // 2)
            if l % 2 == 1 and l // 2 < cfg.n_odd:
                pool(l // 2)
            ffn(l, 2)

    def t_alloc(self):
        A, cfg = self.arena, self.cfg
        TT = cfg.TT
        A.reset(self.const_words)
        self.hs = A.f32(NCH, TT)
        self.ub = A.bf16(NCH, TT)
        self.xb = A.bf16(NCH, TT)
        self.aT = A.bf16(cfg.NJ, TT)
        self.wb = [A.bf16(8192) for _ in range(4)]
        self.wrr = 0
        self.sq = [A.bf16(TT) for _ in range(4)]
        self.rstd = A.f32(TT)
        self.sg = [A.f32(TT) for _ in range(2)]
        self.uext = [A.f32(TT + 15) for _ in range(2)]
        self.ptmp = [A.f32(TT + 15) for _ in range(2)]
        self.halo = A.f32(NCH, 15)
        self.ost = A.f32(TT)

    def next_wb(self):
        i = self.wrr % 4
        self.wrr += 1
        return i

    def t_norm_stats(self, n):
        c = self.c
        for ch in range(NCH):
            sq = self.sq[ch % 4]
            stag = "sq%d" % (ch % 4)
            if ch % 2 == 0:
                c.I("act", "activation", out=sq[:, 0:n], in_=self.hs[:, ch, 0:n], func=AF.Square, reads=["hs%d" % ch], writes=[stag])
            else:
                c.I("dve", "tensor_tensor", out=sq[:, 0:n], in0=self.hs[:, ch, 0:n], in1=self.hs[:, ch, 0:n], op=ALU.mult, reads=["hs%d" % ch], writes=[stag])
            c.I("pe", "matmul", self.ps[0][:, 0:n], self.onesDb, sq[:, 0:n], start=(ch == 0), stop=(ch == NCH - 1), reads=[stag, "cst"], writes=["ps0"])
        c.op("act", lambda e: e.activation(out=self.rstd[:, 0:n], in_=self.ps[0][:, 0:n], func=AF.Sqrt, bias=self.epsc[:, 0:1], scale=1.0),
             reads=["ps0"], writes=["rstd"])
        c.op("dve", lambda e: e.reciprocal(out=self.rstd[:, 0:n], in_=self.rstd[:, 0:n]), reads=["rstd"], writes=["rstd"])

    def t_norm_apply(self, n, gidx, dst, dtag):
        c = self.c
        for ch in range(NCH):
            eng = "dve"
            c.op(eng, lambda e, ch=ch: e.scalar_tensor_tensor(out=dst[:, ch, 0:n], in0=self.hs[:, ch, 0:n], scalar=self.gains[:, gidx, ch:ch + 1],
                                                            in1=self.rstd[:, 0:n], op0=ALU.mult, op1=ALU.mult),
                 reads=["hs%d" % ch, "rstd"], writes=["%s%d" % (dtag, ch)])

    def t_ffn(self, n, l, s):
        c, cfg = self.c, self.cfg
        self.t_norm_stats(n)
        self.t_norm_apply(n, self.gidx[("ffn%d" % s, l)], self.ub, "ub")
        WG, WU, WD = self.WG[(l, s)], self.WU[(l, s)], self.WD[(l, s)]
        ubtags = ["ub%d" % ch for ch in range(NCH)]
        for jb in range(cfg.JB):
            ig = self.next_wb()
            iu = self.next_wb()
            wg = self.wb[ig].rearrange("p (c f) -> p c f", f=512)
            wu = self.wb[iu].rearrange("p (c f) -> p c f", f=512)
            c.dma("sp", self.wb[ig], WG[jb], reads=["WG%d%d_%d" % (l, s, jb)], writes=["wb%d" % ig])
            c.dma("sp", self.wb[iu], WU[jb], reads=["WU%d%d_%d" % (l, s, jb)], writes=["wb%d" % iu])
            for jj in range(4):
                j = jb * 4 + jj
                pg = j % 2
                pu = 2 + j % 2
                for ch in range(NCH):
                    c.op("pe", lambda e, ch=ch, jj=jj, pg=pg, wg=wg: e.matmul(self.ps[pg][:, 0:n], wg[:, ch, jj * 128:(jj + 1) * 128], self.ub[:, ch, 0:n],
                                                                             start=(ch == 0), stop=(ch == NCH - 1)),
                         reads=["wb%d" % ig, "ub%d" % ch], writes=["ps%d" % pg])
                for ch in range(NCH):
                    c.op("pe", lambda e, ch=ch, jj=jj, pu=pu, wu=wu: e.matmul(self.ps[pu][:, 0:n], wu[:, ch, jj * 128:(jj + 1) * 128], self.ub[:, ch, 0:n],
                                                                             start=(ch == 0), stop=(ch == NCH - 1)),
                         reads=["wb%d" % iu, "ub%d" % ch], writes=["ps%d" % pu])
                sg = self.sg[j % 2]
                c.op("act", lambda e, sg=sg, pg=pg: e.activation(out=sg[:, 0:n], in_=self.ps[pg][:, 0:n], func=AF.Silu),
                     reads=["ps%d" % pg], writes=["sg%d" % (j % 2)])
                c.op("dve", lambda e, sg=sg, pu=pu, j=j: e.tensor_tensor(out=self.aT[:, j, 0:n], in0=self.ps[pu][:, 0:n], in1=sg[:, 0:n], op=ALU.mult),
                     reads=["ps%d" % pu, "sg%d" % (j % 2)], writes=["aT%d" % j])
        for mb in range(4):
            for jg in range(cfg.JG):
                iw = self.next_wb()
                wd = self.wb[iw][:, 0:cfg.JQ * 512].rearrange("p (j d) -> p j d", d=512)
                c.dma(self.dq(), self.wb[iw][:, 0:cfg.JQ * 512], WD[mb, jg], reads=["WD%d%d_%d_%d" % (l, s, mb, jg)], writes=["wb%d" % iw])
                for jq in range(cfg.JQ):
                    j = jg * cfg.JQ + jq
                    for mi in range(4):
                        c.op("pe", lambda e, jq=jq, mi=mi, j=j, wd=wd: e.matmul(self.ps[4 + mi][:, 0:n], wd[:, jq, mi * 128:(mi + 1) * 128], self.aT[:, j, 0:n],
                                                                               start=(j == 0), stop=(j == cfg.NJ - 1)),
                             reads=["wb%d" % iw, "aT%d" % j], writes=["ps%d" % (4 + mi)])
            for mi in range(4):
                m = mb * 4 + mi
                c.op("dve", lambda e, mi=mi, m=m: e.scalar_tensor_tensor(out=self.hs[:, m, 0:n], in0=self.ps[4 + mi][:, 0:n], scalar=0.5, in1=self.hs[:, m, 0:n],
                                                                        op0=ALU.mult, op1=ALU.add),
                     reads=["ps%d" % (4 + mi), "hs%d" % m], writes=["hs%d" % m])

    def t_wout(self, n, e_idx, t0):
        c = self.c
        ysrc = self.yT.rearrange("(c p) s -> p c s", p=128)[:, :, t0:t0 + n]
        c.dma("sp", self.xb[:, :, 0:n], ysrc, reads=["yT"], writes=["xb%d" % ch for ch in range(NCH)])
        for mb in range(4):
            iw = self.next_wb()
            w = self.wb[iw].rearrange("p (c f) -> p c f", f=512)
            c.dma(self.dq(), self.wb[iw], self.WOUT[e_idx][mb], reads=["WOUT%d_%d" % (e_idx, mb)], writes=["wb%d" % iw])
            for mi in range(4):
                for ch in range(NCH):
                    c.op("pe", lambda e, ch=ch, mi=mi, w=w: e.matmul(self.ps[4 + mi][:, 0:n], w[:, ch, mi * 128:(mi + 1) * 128], self.xb[:, ch, 0:n],
                                                                    start=(ch == 0), stop=(ch == NCH - 1)),
                         reads=["wb%d" % iw, "xb%d" % ch], writes=["ps%d" % (4 + mi)])
                m = mb * 4 + mi
                c.op("dve", lambda e, mi=mi, m=m: e.tensor_tensor(out=self.hs[:, m, 0:n], in0=self.ps[4 + mi][:, 0:n], in1=self.hs[:, m, 0:n], op=ALU.add),
                     reads=["ps%d" % (4 + mi), "hs%d" % m], writes=["hs%d" % m])

    def t_pool(self, n, l, first_tile):
        c = self.c
        o = l // 2
        self.t_norm_stats(n)
        gidx = self.gidx[("mix", l)]
        iw = self.next_wb()
        pw = self.wb[iw].rearrange("p (g ci co) -> p g ci co", ci=4, co=512)
        c.dma(self.dq(), self.wb[iw], self.PW[o], reads=["PW%d" % o], writes=["wb%d" % iw])
        for gi, win in enumerate((2, 4, 8, 16)):
            for ci in range(4):
                ch = gi * 4 + ci
                ue = self.uext[ch % 2]
                utag = "uext%d" % (ch % 2)
                if first_tile:
                    c.op("pool", lambda e, ue=ue: e.memset(ue[:, 0:15], 0.0), writes=[utag])
                else:
                    c.op("pool", lambda e, ue=ue, ch=ch: e.tensor_copy(out=ue[:, 0:15], in_=self.halo[:, ch, :]), reads=["halo%d" % ch], writes=[utag])
                c.op("dve", lambda e, ue=ue, ch=ch: e.scalar_tensor_tensor(out=ue[:, 15:15 + n], in0=self.hs[:, ch, 0:n], scalar=self.gains[:, gidx, ch:ch + 1],
                                                                          in1=self.rstd[:, 0:n], op0=ALU.mult, op1=ALU.mult),
                     reads=["hs%d" % ch, "rstd", utag], writes=[utag])
                c.op("pool", lambda e, ue=ue, ch=ch: e.tensor_copy(out=self.halo[:, ch, :], in_=ue[:, n:n + 15]), reads=[utag], writes=["halo%d" % ch])
                src, stag = ue, utag
                step = 1
                k = 0
                while step < win:
                    dst = self.ptmp[k % 2]
                    dtag = "ptmp%d" % (k % 2)
                    lo = 2 * step - 1
                    eng = self.ew()
                    c.op(eng, lambda e, src=src, dst=dst, lo=lo, step=step: e.tensor_tensor(out=dst[:, lo:15 + n], in0=src[:, lo:15 + n], in1=src[:, lo - step:15 + n - step], op=ALU.add),
                         reads=[stag], writes=[dtag])
                    src, stag = dst, dtag
                    step *= 2
                    k += 1
                eng = "dve"
                c.op(eng, lambda e, src=src, ue=ue, ch=ch, win=win: e.scalar_tensor_tensor(out=self.xb[:, ch, 0:n], in0=src[:, 15:15 + n], scalar=1.0 / win, in1=ue[:, 15:15 + n],
                                                                                          op0=ALU.mult, op1=ALU.subtract),
                     reads=[stag, utag], writes=["xb%d" % ch])
                if first_tile:
                    m = min(16, n)
                    fx = self.ptmp[k % 2]
                    ftag = "ptmp%d" % (k % 2)
                    c.op("dve", lambda e, src=src, fx=fx, gi=gi, m=m: e.tensor_tensor(out=fx[:, 0:m], in0=src[:, 15:15 + m], in1=self.invcnt[:, gi, 0:m], op=ALU.mult),
                         reads=[stag], writes=[ftag])
                    c.op("dve", lambda e, fx=fx, ue=ue, ch=ch, m=m: e.tensor_tensor(out=self.xb[:, ch, 0:m], in0=fx[:, 0:m], in1=ue[:, 15:15 + m], op=ALU.subtract),
                         reads=[ftag, utag, "xb%d" % ch], writes=["xb%d" % ch])
            for co in range(4):
                for ci in range(4):
                    c.op("pe", lambda e, gi=gi, ci=ci, co=co: e.matmul(self.ps[4 + co][:, 0:n], pw[:, gi, ci, co * 128:(co + 1) * 128], self.xb[:, gi * 4 + ci, 0:n],
                                                                      start=(ci == 0), stop=(ci == 3)),
                         reads=["wb%d" % iw, "xb%d" % (gi * 4 + ci)], writes=["ps%d" % (4 + co)])
            for co in range(4):
                m = gi * 4 + co
                c.op("dve", lambda e, co=co, m=m: e.scalar_tensor_tensor(out=self.hs[:, m, 0:n], in0=self.ps[4 + co][:, 0:n], scalar=self.pscale[:, o, m:m + 1],
                                                                        in1=self.hs[:, m, 0:n], op0=ALU.mult, op1=ALU.add),
                     reads=["ps%d" % (4 + co), "hs%d" % m], writes=["hs%d" % m])


    def m_alloc_moba(self):
        A, cfg = self.arena, self.cfg
        S = cfg.S
        A.reset(self.const_words)
        self.QT = A.bf16(4, S)
        self.KT = A.bf16(4, S)
        self.V = A.bf16(S // 128, 512)
        self.maskT = A.bf16(S)
        self.sel = A.bf16(64, 128)
        self.mut = A.bf16(NCH, 512)
        self.wbm = [A.bf16(8192) for _ in range(2)]
        self.qf = A.f32(4, 512)
        self.kmT = A.f32(4, 16)
        self.gs = A.f32(64)
        self.mk = A.f32(64)
        self.mx = A.f32(32)
        self.pT = [A.bf16(256) for _ in range(4)]
        self.rc = A.f32(256)
        self.yo = [A.bf16(256) for _ in range(2)]
        self.identB = A.bf16(128)
        self.triB = A.bf16(128)
        self.onesB = A.bf16(128)
        c = self.c
        c.op("dve", lambda e: e.tensor_copy(out=self.identB, in_=self.ident), reads=["cst"], writes=["mconst"])
        c.op("dve", lambda e: e.tensor_copy(out=self.triB, in_=self.tri_f), reads=["cst"], writes=["mconst"])
        c.op("pool", lambda e: e.memset(self.onesB, 1.0), writes=["mconst"])
        c.op("dve", lambda e: e.tensor_copy(out=self.sel[0:64], in_=self.ident[0:64, 0:64].unsqueeze(2).to_broadcast([64, 64, 128])),
             reads=["cst"], writes=["mconst"])
        c.op("pool", lambda e: e.memset(self.kmT, 0.0), writes=["kmT"])

    def m_moba(self, e_idx):
        c, cfg = self.c, self.cfg
        S, NT = cfg.S, cfg.S // 512
        NB = S // 256
        WIN = self.WIN[e_idx]
        uTv = self.uT.rearrange("(c p) s -> p c s", p=128)
        scale = 128.0 ** -0.5
        allut = ["ut%d" % ch for ch in range(NCH)]
        wrr = [0]

        def loadw(cb):
            i = wrr[0] % 2
            wrr[0] += 1
            c.dma("sp", self.wbm[i], WIN[cb], reads=["WIN%d_%d" % (e_idx, cb)], writes=["wbm%d" % i])
            return i, self.wbm[i].rearrange("p (c f) -> p c f", f=512)

        for hg in range(2):
            for t in range(NT):
                t0 = t * 512
                c.dma("sp", self.mut, uTv[:, :, t0:t0 + 512], reads=["uT"], writes=allut)
                iq, wq = loadw(hg)
                for hh in range(4):
                    pb = hh % 2
                    for ch in range(NCH):
                        c.op("pe", lambda e, ch=ch, hh=hh, pb=pb, wq=wq: e.matmul(self.ps[pb][:, 0:512], wq[:, ch, hh * 128:(hh + 1) * 128], self.mut[:, ch, :],
                                                                                 start=(ch == 0), stop=(ch == NCH - 1)),
                             reads=["wbm%d" % iq, "ut%d" % ch], writes=["ps%d" % pb])
                    c.op("act", lambda e, hh=hh, pb=pb, t0=t0: e.activation(out=self.QT[:, hh, t0:t0 + 512], in_=self.ps[pb][:, 0:512], func=AF.Copy, scale=scale),
                         reads=["ps%d" % pb], writes=["QT%d" % hh])
                    c.op("dve", lambda e, hh=hh, pb=pb: e.tensor_copy(out=self.qf[:, hh, :], in_=self.ps[pb][:, 0:512]),
                         reads=["ps%d" % pb], writes=["qf"])
                ik, wk = loadw(2 + hg)
                for hh in range(4):
                    pb = 2 + hh % 2
                    for ch in range(NCH):
                        c.op("pe", lambda e, ch=ch, hh=hh, pb=pb, wk=wk: e.matmul(self.ps[pb][:, 0:512], wk[:, ch, hh * 128:(hh + 1) * 128], self.mut[:, ch, :],
                                                                                 start=(ch == 0), stop=(ch == NCH - 1)),
                             reads=["wbm%d" % ik, "ut%d" % ch], writes=["ps%d" % pb])
                    c.op("act", lambda e, hh=hh, pb=pb, t0=t0: e.activation(out=self.KT[:, hh, t0:t0 + 512], in_=self.ps[pb][:, 0:512], func=AF.Copy),
                         reads=["ps%d" % pb], writes=["KT%d" % hh])
                    c.op("dve", lambda e, hh=hh, pb=pb, t=t: e.reduce_sum(out=self.kmT[:, hh, 2 * t:2 * t + 2], in_=self.ps[pb][:, 0:512].rearrange("p (b k) -> p b k", k=256), axis=AX.X),
                         reads=["ps%d" % pb], writes=["kmT"])
                iv, wv = loadw(4 + hg)
                for sub in range(4):
                    pb = 4 + sub % 2
                    for ch in range(NCH):
                        c.op("pe", lambda e, ch=ch, sub=sub, pb=pb, wv=wv: e.matmul(self.ps[pb][:, 0:512], self.mut[:, ch, sub * 128:(sub + 1) * 128], wv[:, ch, :],
                                                                                   start=(ch == 0), stop=(ch == NCH - 1)),
                             reads=["wbm%d" % iv, "ut%d" % ch], writes=["ps%d" % pb])
                    c.op("act", lambda e, sub=sub, pb=pb, t=t: e.activation(out=self.V[:, 4 * t + sub, :], in_=self.ps[pb][:, 0:512], func=AF.Copy),
                         reads=["ps%d" % pb], writes=["V"])
                for sub in range(4):
                    blk = 2 * t + sub // 2
                    if blk == 0 or DBG.get("nogate"):
                        continue
                    for hh in range(4):
                        c.op("pe", lambda e, hh=hh, sub=sub: e.matmul(self.ps[6][:, hh * 16:(hh + 1) * 16], self.qf[:, hh, sub * 128:(sub + 1) * 128], self.kmT[:, hh, :],
                                                                     start=True, stop=True),
                             reads=["qf", "kmT"], writes=["ps6"])
                    c.op("pool", lambda e: e.memset(self.gs, -1e30), writes=["gs"])
                    c.op("dve", lambda e, blk=blk: e.tensor_copy(out=self.gs.rearrange("p (h n) -> p h n", n=16)[:, :, 0:blk],
                                                                in_=self.ps[6][:, 0:64].rearrange("p (h n) -> p h n", n=16)[:, :, 0:blk]),
                         reads=["ps6", "gs"], writes=["gs"])
                    for hh in range(4):
                        c.op("dve", lambda e, hh=hh: e.max(out=self.mx[:, hh * 8:(hh + 1) * 8], in_=self.gs[:, hh * 16:(hh + 1) * 16]),
                             reads=["gs"], writes=["mx"])
                    for hh in range(4):
                        c.op("dve", lambda e, hh=hh: e.tensor_scalar(out=self.mk[:, hh * 16:(hh + 1) * 16], in0=self.gs[:, hh * 16:(hh + 1) * 16],
                                                                    scalar1=self.mx[:, hh * 8 + 2:hh * 8 + 3], scalar2=1.0, op0=ALU.is_ge, op1=ALU.subtract),
                             reads=["gs", "mx"], writes=["mk"])
                    c.op("pe", lambda e: e.matmul(self.ps[7][0:64, 0:128], self.mk, self.ident, start=True, stop=True),
                         reads=["mk", "cst"], writes=["ps7"])
                    q0 = t0 + sub * 128
                    c.op("act", lambda e, q0=q0: e.activation(out=self.maskT[0:64, q0:q0 + 128], in_=self.ps[7][0:64, 0:128], func=AF.Copy, scale=-NEG),
                         reads=["ps7"], writes=["maskT"])
            I = c.I
            PCB = [0, 1, 6, 7]
            steps = []
            for hh in range(0 if DBG.get("noattn") else 4):
                for b in range(NB):
                    for kt in range(2 * b + 2):
                        steps.append((hh, b, kt))

            def emit_qk(i):
                hh, b, kt = steps[i]
                pc = PCB[i % 4]
                q0 = b * 256
                k0 = kt * 128
                nb = kt // 2
                if nb < b:
                    I("pe", "matmul", self.ps[pc][:, 0:256], self.KT[:, hh, k0:k0 + 128], self.QT[:, hh, q0:q0 + 256], start=True, stop=False,
                      reads=["KT%d" % hh, "QT%d" % hh], writes=["ps%d" % pc])
                    r = hh * 16 + nb
                    I("pe", "matmul", self.ps[pc][:, 0:256], self.sel[0:64, r, :], self.maskT[0:64, q0:q0 + 256], start=False, stop=True,
                      reads=["mconst", "maskT"], writes=["ps%d" % pc])
                elif kt == 2 * b:
                    I("pe", "matmul", self.ps[pc][:, 0:256], self.KT[:, hh, k0:k0 + 128], self.QT[:, hh, q0:q0 + 256], start=True, stop=False,
                      reads=["KT%d" % hh, "QT%d" % hh], writes=["ps%d" % pc])
                    I("pe", "matmul", self.ps[pc][:, 0:128], self.identB, self.triB, start=False, stop=True, reads=["mconst"], writes=["ps%d" % pc])
                else:
                    I("pe", "matmul", self.ps[pc][:, 128:256], self.KT[:, hh, k0:k0 + 128], self.QT[:, hh, q0 + 128:q0 + 256], start=True, stop=False,
                      reads=["KT%d" % hh, "QT%d" % hh], writes=["ps%d" % pc])
                    I("pe", "matmul", self.ps[pc][:, 128:256], self.identB, self.triB, start=False, stop=True, reads=["mconst"], writes=["ps%d" % pc])

            def emit_rest(i):
                hh, b, kt = steps[i]
                pc = PCB[i % 4]
                pT = self.pT[i % 4]
                ptag = "pT%d" % (i % 4)
                pO = 2 + b % 2
                pS = 4 + b % 2
                q0 = b * 256
                last = 2 * b + 1
                lo = 128 if kt == last else 0
                I("act", "activation", out=pT[:, lo:256], in_=self.ps[pc][:, lo:256], func=AF.Exp, reads=["ps%d" % pc], writes=[ptag])
                I("pe", "matmul", self.ps[pO][:, lo:256], self.V[:, kt, hh * 128:(hh + 1) * 128], pT[:, lo:256], start=(kt == 0), stop=(kt == last),
                  reads=["V", ptag], writes=["ps%d" % pO])
                I("pe", "matmul", self.ps[pS][:, lo:256], self.onesB, pT[:, lo:256], start=(kt == 0), stop=(kt == last),
                  reads=["mconst", ptag], writes=["ps%d" % pS])
                if kt == last:
                    yo = self.yo[b % 2]
                    I("dve", "reciprocal", out=self.rc, in_=self.ps[pS][:, 0:256], reads=["ps%d" % pS], writes=["rc"])
                    I("dve", "tensor_tensor", out=yo, in0=self.ps[pO][:, 0:256], in1=self.rc, op=ALU.mult, reads=["ps%d" % pO, "rc"], writes=["yo%d" % (b % 2)])
                    hrow = (hg * 4 + hh) * 128
                    c.dma("sp", self.yT[hrow:hrow + 128, q0:q0 + 256], yo, reads=["yo%d" % (b % 2)], writes=["yT"])

            LA = 2
            for i in range(min(LA, len(steps))):
                emit_qk(i)
            for i in range(len(steps)):
                if i + LA < len(steps):
                    emit_qk(i + LA)
                emit_rest(i)


    def m_alloc_rwkv(self):
        A, cfg, c = self.arena, self.cfg, self.c
        A.reset(self.const_words)
        self.rw_w = [A.bf16(8192) for _ in range(3)]
        self.rw_wl = A.bf16(16 * 192)
        self.ut = A.bf16(NCH, 512)
        self.w2s = A.f32(1024)
        self.a2s = A.f32(1024)
        self.g2s = A.f32(1024)
        names = ["mu_r", "mu_k", "mu_v", "omu_r", "omu_k", "omu_v", "w0", "a0", "k_k", "k_a", "omka", "r_k", "gn_g", "gn_b"]
        self.pp = {nm: A.f32(8) for nm in names}
        self.mu_l = A.f32(3)
        self.omu_l = A.f32(3)
        self.carry = A.f32(32)
        self.Tst = [A.f32(8, 64) for _ in range(2)]
        self.tw = A.f32(512)
        self.al = A.f32(512)
        self.sgl = A.f32(512)
        self.zbuf = [A.f32(513) for _ in range(2)]
        for nm in ["R", "K", "V", "LW", "A_", "G", "KK", "T1", "BON", "CUM", "EP", "RW", "YR", "G0T", "H0P"]:
            setattr(self, "b_" + nm, A.f32(512))
        self.tm = A.bf16(4, 4, 128)
        for hs in range(2):
            setattr(self, "h%d_MT" % hs, A.f32(512))
            for nm in ["X0", "X1", "Xt0", "Xt1", "MTb", "Aak", "Arb", "Ark", "T1s", "UW"]:
                setattr(self, "h%d_%s" % (hs, nm), A.bf16(512))
        for nm in ["Rb", "Kb", "Bb", "Ab", "Vb"]:
            setattr(self, "b_" + nm, A.bf16(512))
        self.rw_identB = A.bf16(128)
        c.I("dve", "tensor_copy", out=self.rw_identB, in_=self.ident, reads=["cst"], writes=["rconst"])
        self.maskU4 = A.f32(512)
        self.maskUe4 = A.f32(512)
        self.maskL4 = A.f32(512)
        self.ident4 = A.f32(512)
        self.istack8 = A.f32(512)
        self.yob = A.bf16(512)
        for i in range(4):
            cs = slice(i * 128, (i + 1) * 128)
            c.I("pool", "tensor_copy", out=self.maskU4[:, cs], in_=self.maskU, reads=["cst"], writes=["rconst"])
            c.I("pool", "tensor_copy", out=self.maskUe4[:, cs], in_=self.maskUe, reads=["cst"], writes=["rconst"])
            c.I("pool", "tensor_copy", out=self.maskL4[:, cs], in_=self.maskL, reads=["cst"], writes=["rconst"])
            c.I("pool", "tensor_copy", out=self.ident4[:, cs], in_=self.ident, reads=["cst"], writes=["rconst"])
        c.I("pool", "tensor_tensor", out=self.istack8[:, 0:64], in0=self.ident[:, 0:64], in1=self.ident[:, 64:128], op=ALU.add, reads=["cst"], writes=["rconst"])
        for i in range(1, 8):
            c.I("pool", "tensor_copy", out=self.istack8[:, i * 64:(i + 1) * 64], in_=self.istack8[:, 0:64], reads=["rconst"], writes=["rconst"])

    def m_rwkv_params(self, e):
        c = self.c
        inp = self.inp
        pp = self.pp

        def ld(dst, src1d):
            c.dma("sp", dst, src1d.rearrange("(hp p) -> p hp", p=128), writes=["rparam"])

        mu = inp["rwkv_mu"][e]
        ld(pp["mu_r"], mu[0:1024])
        ld(pp["mu_k"], mu[1024:2048])
        ld(pp["mu_v"], mu[2048:3072])
        c.dma("sp", self.mu_l[0:64, :], mu[3072:3264].rearrange("(q p) -> p q", p=64), writes=["rparam"])
        ld(pp["w0"], inp["rwkv_w0"][e])
        ld(pp["a0"], inp["rwkv_a0"][e])
        ld(pp["k_k"], inp["rwkv_k_k"][e])
        ld(pp["k_a"], inp["rwkv_k_a"][e])
        ld(pp["r_k"], inp["rwkv_r_k"][e].rearrange("h d -> (h d)"))
        ld(pp["gn_g"], inp["rwkv_gn_g"][e])
        ld(pp["gn_b"], inp["rwkv_gn_b"][e])
        c.dma("sp", self.w2s[0:64, :], inp["rwkv_w2"][e], writes=["rparam"])
        c.dma("sp", self.a2s[0:64, :], inp["rwkv_a2"][e], writes=["rparam"])
        c.dma("sp", self.g2s[0:64, :], inp["rwkv_g2"][e], writes=["rparam"])
        for a, b in (("omu_r", "mu_r"), ("omu_k", "mu_k"), ("omu_v", "mu_v"), ("omka", "k_a")):
            c.I("dve", "tensor_scalar", out=pp[a], in0=pp[b], scalar1=-1.0, scalar2=1.0, op0=ALU.mult, op1=ALU.add, reads=["rparam"], writes=["rparam"])
        c.I("dve", "tensor_scalar", out=self.omu_l[0:64, :], in0=self.mu_l[0:64, :], scalar1=-1.0, scalar2=1.0, op0=ALU.mult, op1=ALU.add, reads=["rparam"], writes=["rparam"])

    def _lerp(self, psb, P, cidx, mu, omu, out, otag, first):
        c = self.c
        self._zi = getattr(self, "_zi", 0) + 1
        zi = self._zi % 2
        zb = self.zbuf[zi]
        zt = "zbuf%d" % zi
        ctag = "carry%d" % cidx
        c.I("act", "activation", out=zb[0:P, 1:513], in_=self.ps[psb][0:P, 0:512], func=AF.Copy, reads=["ps%d" % psb], writes=[zt])
        if first:
            c.I("pool", "memset", zb[0:P, 0:1], 0.0, writes=[zt])
        else:
            c.I("pool", "tensor_copy", out=zb[0:P, 0:1], in_=self.carry[0:P, cidx:cidx + 1], reads=[ctag], writes=[zt])
        c.I("pool", "tensor_copy", out=self.carry[0:P, cidx:cidx + 1], in_=zb[0:P, 512:513], reads=[zt], writes=[ctag])
        c.I("dve", "tensor_scalar", out=out[0:P, :], in0=zb[0:P, 0:512], scalar1=mu, scalar2=None, op0=ALU.mult, reads=[zt, "rparam"], writes=[otag])
        c.I("dve", "scalar_tensor_tensor", out=out[0:P, :], in0=zb[0:P, 1:513], scalar=omu, in1=out[0:P, :], op0=ALU.mult, op1=ALU.add,
            reads=[zt, "rparam", otag], writes=[otag])

    def m_rwkv(self, e_idx):
        c, cfg = self.c, self.cfg
        S, NT = cfg.S, cfg.S // 512
        WIN = self.WIN[e_idx]
        uTv = self.uT.rearrange("(c p) s -> p c s", p=128)
        pp = self.pp
        ps = self.ps
        I = c.I
        B = lambda nm: getattr(self, "b_" + nm)
        H = lambda nm: getattr(self, "h_" + nm)
        allut = ["ut%d" % ch for ch in range(NCH)]
        self.m_rwkv_params(e_idx)
        c.dma("sp", self.rw_wl, WIN[12][:, 0:16 * 192], reads=["WIN%d_12" % e_idx], writes=["rw_wl"])
        wl = self.rw_wl.rearrange("p (c f) -> p c f", f=192)
        gchunk = 0
        for pg in range(2):
            for q in range(3):
                c.dma("sp", self.rw_w[q], WIN[6 + 2 * q + pg], reads=["WIN%d_%d" % (e_idx, 6 + 2 * q + pg)], writes=["rw_w%d" % q])
            wq3 = [w.rearrange("p (c f) -> p c f", f=512) for w in self.rw_w]
            for t in range(NT):
                t0 = t * 512
                first = (t == 0)
                c.dma("sp", self.ut, uTv[:, :, t0:t0 + 512], reads=["uT"], writes=allut)
                for q, (dst, dtag, fn) in enumerate(((self.tw, "tw", AF.Tanh), (self.al, "al", AF.Copy), (self.sgl, "sgl", AF.Sigmoid))):
                    for ch in range(NCH):
                        I("pe", "matmul", ps[0][0:64, 0:512], wl[:, ch, q * 64:(q + 1) * 64], self.ut[:, ch, :], start=(ch == 0), stop=(ch == NCH - 1),
                          reads=["rw_wl", "ut%d" % ch], writes=["ps0"])
                    self._lerp(0, 64, 24 + q, self.mu_l[0:64, q:q + 1], self.omu_l[0:64, q:q + 1], dst, dtag, first)
                    if fn != AF.Copy:
                        I("act", "activation", out=dst[0:64, :], in_=dst[0:64, :], func=fn, reads=[dtag], writes=[dtag])
                LVL = int(os.environ.get("RWLVL", "6"))
                for pl in range(4 if LVL >= 2 else 0):
                    hp = pg * 4 + pl
                    col = slice(hp, hp + 1)
                    for q, (nm, mun) in enumerate((("R", "r"), ("K", "k"), ("V", "v"))):
                        pb = q % 2
                        for ch in range(NCH):
                            I("pe", "matmul", ps[pb][:, 0:512], wq3[q][:, ch, pl * 128:(pl + 1) * 128], self.ut[:, ch, :], start=(ch == 0), stop=(ch == NCH - 1),
                              reads=["rw_w%d" % q, "ut%d" % ch], writes=["ps%d" % pb])
                        self._lerp(pb, 128, q * 8 + hp, pp["mu_" + mun][:, col], pp["omu_" + mun][:, col], B(nm), "b_" + nm, first)
                    R, K, V, LW, A_, G, KK, T1, BON, CUM, EP = (B(x) for x in ("R", "K", "V", "LW", "A_", "G", "KK", "T1", "BON", "CUM", "EP"))
                    cs128 = slice(hp * 128, (hp + 1) * 128)
                    I("pe", "matmul", ps[0][:, 0:512], self.w2s[0:64, cs128], self.tw[0:64, :], start=True, stop=True, reads=["rparam", "tw"], writes=["ps0"])
                    I("act", "activation", out=LW, in_=ps[0][:, 0:512], func=AF.Sigmoid, bias=pp["w0"][:, col], scale=1.0, reads=["ps0", "rparam"], writes=["b_LW"])
                    I("pool", "tensor_scalar", out=LW, in0=LW, scalar1=-math.exp(-0.5), scalar2=None, op0=ALU.mult, reads=["b_LW"], writes=["b_LW"])
                    I("pe", "matmul", ps[1][:, 0:512], self.a2s[0:64, cs128], self.al[0:64, :], start=True, stop=True, reads=["rparam", "al"], writes=["ps1"])
                    I("act", "activation", out=A_, in_=ps[1][:, 0:512], func=AF.Sigmoid, bias=pp["a0"][:, col], scale=1.0, reads=["ps1", "rparam"], writes=["b_A_"])
                    I("pe", "matmul", ps[0][:, 0:512], self.g2s[0:64, cs128], self.sgl[0:64, :], start=True, stop=True, reads=["rparam", "sgl"], writes=["ps0"])
                    I("act", "activation", out=G, in_=ps[0][:, 0:512], func=AF.Copy, reads=["ps0"], writes=["b_G"])
                    I("dve", "tensor_scalar", out=KK, in0=K, scalar1=pp["k_k"][:, col], scalar2=None, op0=ALU.mult, reads=["b_K", "rparam"], writes=["b_KK"])
                    I("pool", "tensor_tensor", out=T1, in0=KK, in1=KK, op=ALU.mult, reads=["b_KK"], writes=["b_T1"])
                    I("pe", "matmul", ps[1][:, 0:512], self.bones, T1, start=True, stop=True, reads=["cst", "b_T1"], writes=["ps1"])
                    I("dve", "tensor_scalar", out=T1, in0=ps[1][:, 0:512], scalar1=1e-24, scalar2=None, op0=ALU.max, reads=["ps1"], writes=["b_T1"])
                    I("act", "activation", out=T1, in_=T1, func=AF.Sqrt, reads=["b_T1"], writes=["b_T1"])
                    I("dve", "reciprocal", out=T1, in_=T1, reads=["b_T1"], writes=["b_T1"])
                    I("dve", "tensor_tensor", out=KK, in0=KK, in1=T1, op=ALU.mult, reads=["b_KK", "b_T1"], writes=["b_KK"])
                    I("dve", "tensor_scalar", out=T1, in0=A_, scalar1=pp["k_a"][:, col], scalar2=pp["omka"][:, col], op0=ALU.mult, op1=ALU.add,
                      reads=["b_A_", "rparam"], writes=["b_T1"])
                    I("pool", "tensor_tensor", out=K, in0=K, in1=T1, op=ALU.mult, reads=["b_K", "b_T1"], writes=["b_K"])
                    I("pool", "tensor_tensor", out=A_, in0=KK, in1=A_, op=ALU.mult, reads=["b_KK", "b_A_"], writes=["b_A_"])
                    I("dve", "scalar_tensor_tensor", out=T1, in0=R, scalar=pp["r_k"][:, col], in1=K, op0=ALU.mult, op1=ALU.mult, reads=["b_R", "b_K", "rparam"], writes=["b_T1"])
                    I("pe", "matmul", ps[0][:, 0:512], self.bones, T1, start=True, stop=True, reads=["cst", "b_T1"], writes=["ps0"])
                    I("act", "activation", out=BON, in_=ps[0][:, 0:512], func=AF.Copy, reads=["ps0"], writes=["b_BON"])
                    I("dve", "tensor_tensor_scan", out=CUM, data0=self.reset, data1=LW, initial=0.0, op0=ALU.mult, op1=ALU.add, reads=["cst", "b_LW"], writes=["b_CUM"])
                    I("pool", "tensor_tensor", out=LW, in0=CUM, in1=LW, op=ALU.subtract, reads=["b_CUM", "b_LW"], writes=["b_LW"])
                    I("act", "activation", out=EP, in_=CUM, func=AF.Exp, reads=["b_CUM"], writes=["b_EP"])
                    I("act", "activation", out=CUM, in_=CUM, func=AF.Exp, scale=-1.0, reads=["b_CUM"], writes=["b_CUM"])
                    I("act", "activation", out=LW, in_=LW, func=AF.Exp, reads=["b_LW"], writes=["b_LW"])
                    Rb, Kb, Bb, Ab, Vb = (B(x) for x in ("Rb", "Kb", "Bb", "Ab", "Vb"))
                    I("dve", "tensor_tensor", out=R, in0=R, in1=EP, op=ALU.mult, reads=["b_R", "b_EP"], writes=["b_R"])
                    I("pool", "tensor_copy", out=Rb, in_=R, reads=["b_R"], writes=["b_Rb"])
                    I("pool", "tensor_tensor", out=Kb, in0=K, in1=CUM, op=ALU.mult, reads=["b_K", "b_CUM"], writes=["b_Kb"])
                    I("pool", "tensor_tensor", out=Bb, in0=A_, in1=CUM, op=ALU.mult, reads=["b_A_", "b_CUM"], writes=["b_Bb"])
                    I("dve", "scalar_tensor_tensor", out=Ab, in0=KK, scalar=-1.0, in1=LW, op0=ALU.mult, op1=ALU.mult, reads=["b_KK", "b_LW"], writes=["b_Ab"])
                    I("pool", "tensor_copy", out=Vb, in_=V, reads=["b_V"], writes=["b_Vb"])
                    for cp in range(4 if LVL >= 3 else 0):
                        cs = slice(cp * 128, (cp + 1) * 128)
                        for ty, (src, stag) in enumerate(((Vb, "b_Vb"), (Ab, "b_Ab"), (Bb, "b_Bb"), (Kb, "b_Kb"))):
                            I("pe", "matmul", ps[2][:, ty * 128:(ty + 1) * 128], src[:, cs], self.rw_identB, start=True, stop=True, reads=[stag, "rconst"], writes=["ps2"])
                        eng = "act" if cp % 2 else "dve"
                        if eng == "act":
                            I("act", "activation", out=self.tm[:, cp].rearrange("p a b -> p (a b)"), in_=ps[2][:, 0:512], func=AF.Copy, reads=["ps2"], writes=["tm%d" % cp])
                        else:
                            I("dve", "tensor_copy", out=self.tm[:, cp].rearrange("p a b -> p (a b)"), in_=ps[2][:, 0:512], reads=["ps2"], writes=["tm%d" % cp])
                    tmtags = ["tm%d" % i for i in range(4)]
                    RW, YR, G0T, H0P = B("RW"), B("YR"), B("G0T"), B("H0P")
                    if LVL < 4:
                        continue
                    def head_gen(hs):
                        P_ = slice(64 * hs, 64 * hs + 64)
                        hc = P_
                        ba, bb = (0, 1) if hs == 0 else (2, 3)
                        Hh = lambda nm: getattr(self, "h%d_%s" % (hs, nm))
                        tg = lambda nm: "h%d_%s" % (hs, nm)
                        X = [Hh("X0"), Hh("X1")]
                        Xt = [Hh("Xt0"), Hh("Xt1")]
                        MT, MTb, Aak, Arb, Ark, T1s, UW = Hh("MT"), Hh("MTb"), Hh("Aak"), Hh("Arb"), Hh("Ark"), Hh("T1s"), Hh("UW")
                        CS = [slice(cp * 128, (cp + 1) * 128) for cp in range(4)]

                        def amat(bank, l, ltag, r, rtag):
                            for cs in CS:
                                I("pe", "matmul", ps[bank][:, cs], l[P_, cs], r[P_, cs], start=True, stop=True, reads=[ltag, rtag], writes=["ps%d" % bank])

                        amat(ba, Ab, "b_Ab", Bb, "b_Bb")
                        amat(bb, Bb, "b_Bb", Ab, "b_Ab")
                        yield
                        I("dve", "tensor_tensor", out=X[0], in0=ps[ba][:, 0:512], in1=self.maskL4, op=ALU.mult, reads=["ps%d" % ba, "rconst"], writes=[tg("X0")])
                        I("dve", "tensor_tensor", out=Xt[0], in0=ps[bb][:, 0:512], in1=self.maskU4, op=ALU.mult, reads=["ps%d" % bb, "rconst"], writes=[tg("Xt0")])
                        amat(ba, Kb, "b_Kb", Ab, "b_Ab")
                        amat(bb, Bb, "b_Bb", Rb, "b_Rb")
                        yield
                        I("pool", "tensor_tensor", out=MT, in0=Xt[0], in1=self.ident4, op=ALU.add, reads=[tg("Xt0"), "rconst"], writes=[tg("MT")])
                        I("pool", "tensor_copy", out=MTb, in_=MT, reads=[tg("MT")], writes=[tg("MTb")])
                        I("dve", "tensor_tensor", out=Aak, in0=ps[ba][:, 0:512], in1=self.maskU4, op=ALU.mult, reads=["ps%d" % ba, "rconst"], writes=[tg("Aak")])
                        I("dve", "tensor_tensor", out=Arb, in0=ps[bb][:, 0:512], in1=self.maskUe4, op=ALU.mult, reads=["ps%d" % bb, "rconst"], writes=[tg("Arb")])
                        amat(ba, Kb, "b_Kb", Rb, "b_Rb")
                        yield
                        I("dve", "tensor_tensor", out=Ark, in0=ps[ba][:, 0:512], in1=self.maskUe4, op=ALU.mult, reads=["ps%d" % ba, "rconst"], writes=[tg("Ark")])
                        cur = 0
                        for k in range(5):
                            nx = 1 - cur
                            for cs in CS:
                                I("pe", "matmul", ps[bb][:, cs], Xt[cur][:, cs], X[cur][:, cs], start=True, stop=True, reads=[tg("X%d" % cur), tg("Xt%d" % cur)], writes=["ps%d" % bb])
                            if k < 4:
                                for cs in CS:
                                    I("pe", "matmul", ps[ba][:, cs], X[cur][:, cs], Xt[cur][:, cs], start=True, stop=True, reads=[tg("X%d" % cur), tg("Xt%d" % cur)], writes=["ps%d" % ba])
                            yield
                            I("act", "activation", out=X[nx], in_=ps[bb][:, 0:512], func=AF.Copy, reads=["ps%d" % bb], writes=[tg("X%d" % nx)])
                            if k < 4:
                                I("dve", "tensor_copy", out=Xt[nx], in_=ps[ba][:, 0:512], reads=["ps%d" % ba], writes=[tg("Xt%d" % nx)])
                            for cs in CS:
                                I("pe", "matmul", ps[bb][:, cs], X[nx][:, cs], MTb[:, cs], start=True, stop=True, reads=[tg("X%d" % nx), tg("MTb")], writes=["ps%d" % bb])
                            yield
                            I("dve", "tensor_tensor", out=MT, in0=ps[bb][:, 0:512], in1=MT, op=ALU.add, reads=["ps%d" % bb, tg("MT")], writes=[tg("MT")])
                            I("pool", "tensor_copy", out=MTb, in_=MT, reads=[tg("MT")], writes=[tg("MTb")])
                            cur = nx
                        for cp in range(4):
                            I("pe", "matmul", ps[ba][:, cp * 64:(cp + 1) * 64], Aak[:, CS[cp]], self.tm[:, cp, 0, hc], start=True, stop=True, reads=[tg("Aak"), "tm%d" % cp], writes=["ps%d" % ba])
                        yield
                        I("act", "activation", out=T1s[:, 0:256], in_=ps[ba][:, 0:256], func=AF.Copy, reads=["ps%d" % ba], writes=[tg("T1s")])
                        for cp in range(4):
                            I("pe", "matmul", ps[bb][:, cp * 128:cp * 128 + 64], MTb[:, CS[cp]], T1s[:, cp * 64:(cp + 1) * 64], start=True, stop=True, reads=[tg("MTb"), tg("T1s")], writes=["ps%d" % bb])
                            I("pe", "matmul", ps[bb][:, cp * 128 + 64:cp * 128 + 128], MTb[:, CS[cp]], self.tm[:, cp, 1, hc], start=True, stop=True, reads=[tg("MTb"), "tm%d" % cp], writes=["ps%d" % bb])
                        yield
                        I("dve", "tensor_copy", out=UW, in_=ps[bb][:, 0:512], reads=["ps%d" % bb], writes=[tg("UW")])
                        for cp in range(4):
                            I("pe", "matmul", ps[ba][P_, CS[cp]], UW[:, cp * 128 + 64:cp * 128 + 128], Arb[:, CS[cp]], start=True, stop=True, reads=[tg("UW"), tg("Arb")], writes=["ps%d" % ba])
                        for cp in range(4):
                            I("pe", "matmul", ps[5][P_, CS[cp]], UW[:, cp * 128:cp * 128 + 64], Arb[:, CS[cp]], start=(cp == 0), stop=False, skip_group_check=True,
                              reads=[tg("UW"), tg("Arb")], writes=["ps5"])
                            I("pe", "matmul", ps[5][P_, CS[cp]], self.tm[:, cp, 0, hc], Ark[:, CS[cp]], start=False, stop=False, skip_group_check=True,
                              reads=["tm%d" % cp, tg("Ark")], writes=["ps5"])
                        yield
                        I("dve", "tensor_tensor", out=RW[P_, :], in0=ps[ba][P_, 0:512], in1=R[P_, :], op=ALU.add, reads=["ps%d" % ba, "b_R"], writes=["b_RW%d" % hs])

                    gens = [head_gen(0), head_gen(1)]
                    alive = [True, True]
                    while any(alive):
                        for gi in range(2):
                            if alive[gi]:
                                try:
                                    next(gens[gi])
                                except StopIteration:
                                    alive[gi] = False
                    for c2 in range(2):
                        for hs in range(2):
                            P_ = slice(64 * hs, 64 * hs + 64)
                            hc = P_
                            UW = getattr(self, "h%d_UW" % hs)
                            uwt = "h%d_UW" % hs
                            for cp in range(4):
                                qq = cp * 2 + c2
                                TP = slice(64 * c2, 64 * c2 + 64)
                                qs = slice(qq * 64, (qq + 1) * 64)
                                sw = (cp == 0 and hs == 0)
                                I("pe", "matmul", ps[6][P_, qs], UW[TP, cp * 128 + 64:cp * 128 + 128], self.tm[TP, cp, 2, hc], start=True, stop=True, reads=[uwt, "tm%d" % cp], writes=["ps6"], ses=sw)
                                I("pe", "matmul", ps[7][P_, qs], self.tm[TP, cp, 2, hc], UW[TP, cp * 128:cp * 128 + 64], start=(qq == 0), stop=False, skip_group_check=True,
                                  reads=[uwt, "tm%d" % cp], writes=["ps7"], ses=sw)
                                I("pe", "matmul", ps[7][P_, qs], self.tm[TP, cp, 3, hc], self.tm[TP, cp, 0, hc], start=False, stop=True, skip_group_check=True,
                                  reads=["tm%d" % cp], writes=["ps7"])
                    if LVL < 5:
                        continue
                    I("dve", "tensor_tensor", out=G0T, in0=ps[6][:, 0:512], in1=self.istack8, op=ALU.add, reads=["ps6", "rconst"], writes=["b_G0T"])
                    pc8 = EP.rearrange("p (q c) -> p q c", c=64)[:, :, 63:64].to_broadcast([128, 8, 64])
                    I("dve", "tensor_tensor", out=H0P.rearrange("p (q c) -> p q c", c=64), in0=ps[7][:, 0:512].rearrange("p (q c) -> p q c", c=64), in1=pc8, op=ALU.mult,
                      reads=["ps7", "b_EP"], writes=["b_H0P"])
                    for qq in range(8):
                        qs = slice(qq * 64, (qq + 1) * 64)
                        Tc = self.Tst[gchunk % 2]
                        Tn = self.Tst[(gchunk + 1) % 2]
                        tct = "Tst%d_%d" % (gchunk % 2, hp)
                        tnt = "Tst%d_%d" % ((gchunk + 1) % 2, hp)
                        if first and qq == 0:
                            I("pool", "memset", Tc[:, hp, :], 0.0, writes=[tct])
                        for hs in range(2):
                            P_ = slice(64 * hs, 64 * hs + 64)
                            I("pe", "matmul", ps[5][P_, qs], Tc[P_, hp, :], RW[P_, qs], start=False, stop=True, skip_group_check=True, reads=[tct, "b_RW%d" % hs], writes=["ps5"],
                              ses=True)
                            I("pe", "matmul", ps[0][P_, qs], G0T[P_, qs], Tc[P_, hp, :], start=True, stop=True, reads=[tct, "b_G0T"], writes=["ps0"], ses=True)
                        I("dve", "scalar_tensor_tensor", out=Tn[:, hp, :], in0=ps[0][:, qs], scalar=EP[:, qq * 64 + 63:qq * 64 + 64], in1=H0P[:, qs], op0=ALU.mult, op1=ALU.add,
                          reads=["ps0", "b_EP", "b_H0P"], writes=[tnt])
                        gchunk += 1
                    if LVL < 6:
                        continue
                    I("act", "activation", out=YR, in_=ps[5][:, 0:512], func=AF.Copy, reads=["ps5"], writes=["b_YR"])
                    I("pe", "matmul", ps[1][:, 0:512], self.bones, YR, start=True, stop=True, reads=["cst", "b_YR"], writes=["ps1"])
                    I("dve", "scalar_tensor_tensor", out=YR, in0=ps[1][:, 0:512], scalar=-1.0 / 64, in1=YR, op0=ALU.mult, op1=ALU.add, reads=["ps1", "b_YR"], writes=["b_YR"])
                    I("pool", "tensor_tensor", out=T1, in0=YR, in1=YR, op=ALU.mult, reads=["b_YR"], writes=["b_T1"])
                    I("pe", "matmul", ps[1][:, 0:512], self.bones, T1, start=True, stop=True, reads=["cst", "b_T1"], writes=["ps1"])
                    I("act", "activation", out=T1, in_=ps[1][:, 0:512], func=AF.Sqrt, bias=self.epsc[:, 1:2], scale=1.0 / 64, reads=["ps1", "cst"], writes=["b_T1"])
                    I("dve", "reciprocal", out=T1, in_=T1, reads=["b_T1"], writes=["b_T1"])
                    I("dve", "tensor_tensor", out=YR, in0=YR, in1=T1, op=ALU.mult, reads=["b_YR", "b_T1"], writes=["b_YR"])
                    I("dve", "tensor_scalar", out=YR, in0=YR, scalar1=pp["gn_g"][:, col], scalar2=pp["gn_b"][:, col], op0=ALU.mult, op1=ALU.add, reads=["b_YR", "rparam"], writes=["b_YR"])
                    I("pool", "tensor_tensor", out=T1, in0=BON, in1=V, op=ALU.mult, reads=["b_BON", "b_V"], writes=["b_T1"])
                    I("pool", "tensor_tensor", out=YR, in0=YR, in1=T1, op=ALU.add, reads=["b_YR", "b_T1"], writes=["b_YR"])
                    I("dve", "tensor_tensor", out=self.yob, in0=YR, in1=G, op=ALU.mult, reads=["b_YR", "b_G"], writes=["yob"])
                    c.dma("sp", self.yT[1024 + hp * 128:1024 + (hp + 1) * 128, t0:t0 + 512], self.yob, reads=["yob"], writes=["yT"])

    def dbg_dump_y(self, r0, nrows):
        c, cfg = self.c, self.cfg
        A = self.arena
        A.reset(self.const_words)
        tb = A.bf16(cfg.S)
        tf = A.f32(cfg.S)
        evs = []
        for r in range(r0, r0 + nrows, 128):
            c.dma("sp", tb, self.yT[r:r + 128, :], reads=["yT"], writes=["dbgb"])
            c.op("dve", lambda e: e.tensor_copy(out=tf, in_=tb), reads=["dbgb"], writes=["dbgf"])
            evs.append(c.dma("sp", self.outT[r:r + 128, :], tf, reads=["dbgf"], writes=["outd"]))
        return evs

    def t_load(self, src, t0, n):
        v = src.rearrange("(c p) s -> p c s", p=128)[:, :, t0:t0 + n]
        self.c.dma("sp", self.hs[:, :, 0:n], v, reads=["hTd"], writes=["hs%d" % ch for ch in range(NCH)])

    def t_store_h(self, t0, n):
        v = self.hT.rearrange("(c p) s -> p c s", p=128)[:, :, t0:t0 + n]
        self.c.dma("sp", v, self.hs[:, :, 0:n], reads=["hs%d" % ch for ch in range(NCH)], writes=["hTd"])

    def t_store_u(self, l, t0, n):
        self.t_norm_stats(n)
        self.t_norm_apply(n, self.gidx[("mix", l)], self.ub, "ub")
        v = self.uT.rearrange("(c p) s -> p c s", p=128)[:, :, t0:t0 + n]
        self.c.dma("sp", v, self.ub[:, :, 0:n], reads=["ub%d" % ch for ch in range(NCH)], writes=["uT"])

    def t_final(self, t0, n):
        c = self.c
        self.t_norm_stats(n)
        gidx = self.gidx[("final", 0)]
        for ch in range(NCH):
            eng = "dve"
            c.op(eng, lambda e, ch=ch: e.scalar_tensor_tensor(out=self.hs[:, ch, 0:n], in0=self.hs[:, ch, 0:n], scalar=self.gains[:, gidx, ch:ch + 1],
                                                            in1=self.rstd[:, 0:n], op0=ALU.mult, op1=ALU.mult),
                 reads=["hs%d" % ch, "rstd"], writes=["hs%d" % ch])
        v = self.outT.rearrange("(c p) s -> p c s", p=128)[:, :, t0:t0 + n]
        return self.c.dma("sp", v, self.hs[:, :, 0:n], reads=["hs%d" % ch for ch in range(NCH)], writes=["outd"])

    def load_consts(self):
        c, cfg, A = self.c, self.cfg, self.arena
        A.reset(0)
        ncw = CONSTS.shape[1]
        self.cst = A.f32(ncw)
        c.dma("sp", self.cst, self.consts, writes=["cst"])

        def cv(name):
            o, w = COFF[name]
            return self.cst[:, o:o + w]

        self.ident = cv("ident")
        self.tri_f = cv("tri")
        self.bones = cv("bones")
        self.maskU = cv("maskU")
        self.maskUe = cv("maskUe")
        self.maskL = cv("maskL")
        self.reset = cv("reset")
        self.invcnt = cv("invcnt").rearrange("p (g t) -> p g t", t=16)
        names = []
        for l in range(cfg.depth):
            names += [("ffn1", l), ("mix", l), ("ffn2", l)]
        names.append(("final", 0))
        self.gidx = {k: i for i, k in enumerate(names)}
        self.gains = A.f32(len(names), NCH)
        for (k, l), i in self.gidx.items():
            src = self.inp["final_norm"] if k == "final" else self.inp["%s_norm" % k][l]
            c.dma(self.dq(), self.gains[:, i, :], src.rearrange("(c p) -> p c", p=128), writes=["cst"])
        self.pscale = A.f32(max(cfg.n_odd, 1), NCH)
        for o in range(cfg.n_odd):
            c.dma(self.dq(), self.pscale[:, o, :], self.inp["pool_scale"][o].rearrange("(c p) -> p c", p=128), writes=["cst"])
        self.onesD = A.f32(128)
        c.op("pool", lambda e: e.memset(self.onesD, 1.0 / D), writes=["cst"])
        self.onesDb = A.bf16(128)
        c.op("pool", lambda e: e.memset(self.onesDb, 1.0 / D), writes=["cst"])
        self.epsc = A.f32(4)
        c.op("pool", lambda e: e.memset(self.epsc[:, 0:1], 1e-6), writes=["cst"])
        c.op("pool", lambda e: e.memset(self.epsc[:, 1:2], 64e-5), writes=["cst"])
        c.op("pool", lambda e: e.memset(self.epsc[:, 2:3], 0.0), writes=["cst"])
        self.const_words = A.off

    def build(self):
        nc, cfg = self.nc, self.cfg
        self.declare()
        with self.es:
            NW = 52736
            arena_t = self.es.enter_context(nc.sbuf_tensor("arena", [128, NW], F32))
            self.arena = Arena(arena_t, NW)
            self.ps = [self.es.enter_context(nc.psum_tensor("psb%d" % i, [128, 512], F32)) for i in range(8)]
            self.c = Ctx(nc)
            with nc.allow_non_contiguous_dma("small parameter vectors"):
                self.load_consts()
                final = self.program()
                self.c.finish(final)
        return nc

    def program(self):
        c, cfg = self.c, self.cfg
        st = cfg.stages
        self.prep()
        self.t_alloc()
        final = []
        TT = cfg.TT
        if st == "ffn":
            for t in range(cfg.NT):
                self.t_load(self.xT, t * TT, TT)
                self.t_ffn(TT, 0, 1)
                final.append(self.t_final(t * TT, TT))
            return final
        if st == "pool":
            for t in range(cfg.NT):
                self.t_load(self.xT, t * TT, TT)
                self.t_ffn(TT, 1, 1)
                self.t_pool(TT, 1, t == 0)
                self.t_ffn(TT, 1, 2)
                final.append(self.t_final(t * TT, TT))
            return final
        if st == "moba":
            for t in range(cfg.NT):
                self.t_load(self.xT, t * TT, TT)
                self.t_store_u(0, t * TT, TT)
            c.barrier()
            self.m_alloc_moba()
            self.m_moba(0)
            c.barrier()
            return self.dbg_dump_y(0, 1024)
        if st == "rwkv":
            for t in range(cfg.NT):
                self.t_load(self.xT, t * TT, TT)
                self.t_store_u(0, t * TT, TT)
            c.barrier()
            self.m_alloc_rwkv()
            self.m_rwkv(0)
            c.barrier()
            return self.dbg_dump_y(1024, 1024)
        assert cfg.depth == 4
        NT = cfg.NT

        def mixer(e_idx):
            c.barrier()
            self.m_alloc_moba()
            self.m_moba(e_idx)
            c.barrier()
            self.m_alloc_rwkv()
            self.m_rwkv(e_idx)
            c.barrier()
            self.t_alloc()

        for t in range(NT):
            self.t_load(self.xT, t * TT, TT)
            self.t_ffn(TT, 0, 1)
            self.t_store_u(0, t * TT, TT)
            self.t_store_h(t * TT, TT)
        mixer(0)
        for t in range(NT):
            self.t_load(self.hT, t * TT, TT)
            self.t_wout(TT, 0, t * TT)
            self.t_ffn(TT, 0, 2)
            self.t_ffn(TT, 1, 1)
            self.t_pool(TT, 1, t == 0)
            self.t_ffn(TT, 1, 2)
            self.t_ffn(TT, 2, 1)
            self.t_store_u(2, t * TT, TT)
            self.t_store_h(t * TT, TT)
        mixer(1)
        for t in range(NT):
            self.t_load(self.hT, t * TT, TT)
            self.t_wout(TT, 1, t * TT)
            self.t_ffn(TT, 2, 2)
            self.t_ffn(TT, 3, 1)
            self.t_pool(TT, 3, t == 0)
            self.t_ffn(TT, 3, 2)
            final.append(self.t_final(t * TT, TT))
        return final


_CACHE = {}

IN_NAMES = ["ffn1_norm", "ffn1_wg", "ffn1_wu", "ffn1_wd", "mix_norm", "ffn2_norm", "ffn2_wg", "ffn2_wu", "ffn2_wd",
            "ab_w_in", "ab_w_out", "rwkv_mu", "rwkv_w0", "rwkv_w2", "rwkv_a0", "rwkv_a2", "rwkv_g2", "rwkv_k_k", "rwkv_k_a",
            "rwkv_r_k", "rwkv_gn_g", "rwkv_gn_b", "pool_w", "pool_scale", "final_norm"]


def kernel(**inputs):
    x = np.asarray(inputs["x"], dtype=np.float32)
    Bn, S, Dm = x.shape
    assert Dm == D and Bn == 4
    dff = int(np.asarray(inputs["ffn1_wg"]).shape[2])
    depth = int(np.asarray(inputs["ffn1_wg"]).shape[0])
    key = (S, depth, dff)
    if key not in _CACHE:
        _CACHE[key] = Builder(Cfg(S=S, depth=depth, dff=dff)).build()
    nc = _CACHE[key]
    shared = {n: np.ascontiguousarray(np.asarray(inputs[n], dtype=np.float32)) for n in IN_NAMES}
    shared["consts"] = CONSTS
    xT = [np.ascontiguousarray(x[b].T) for b in range(Bn)]
    owner = [0, 1, 4, 5]
    zeros = np.zeros_like(xT[0])
    in_maps = []
    for core in range(8):
        m = dict(shared)
        m["xT"] = xT[owner.index(core)] if core in owner else zeros
        in_maps.append(m)
    res = run_bass_kernel_spmd(nc, in_maps, core_ids=list(range(8)))
    out = np.stack([np.ascontiguousarray(res.results[owner[b]]["outT"].T) for b in range(Bn)], axis=0)
    return out.astype(np.float32)
```
